# Optimizing a Trainium2 kernel written in Bass

```python
import math
import jax
import jax.numpy as jnp
from jax import lax
import numpy as np

D_MODEL = 1024
BATCH = 32
SEQ = 2048
DEPTH = 2

D_MIX = D_MODEL
S5_WIDTH = D_MIX // 2
S5_GROUP_CH = 16
S5_GROUPS = S5_WIDTH // S5_GROUP_CH
S5_STATE = 64
S5_CHUNK = 128
GLA_WIDTH = D_MIX - S5_WIDTH
GLA_HEADS = 4
GLA_DK = GLA_WIDTH // 2
GLA_HEAD_K = GLA_DK // GLA_HEADS
GLA_HEAD_V = GLA_WIDTH // GLA_HEADS
GLA_GATE_RANK = 16
GLA_GATE_TAU = 16.0
GLA_CHUNK = 64
IN_SPLITS = (S5_WIDTH, S5_WIDTH + GLA_DK, S5_WIDTH + 2 * GLA_DK, S5_WIDTH + 2 * GLA_DK + GLA_WIDTH, S5_WIDTH + 2 * GLA_DK + 2 * GLA_WIDTH)
IN_COLS = S5_WIDTH + 2 * GLA_DK + 2 * GLA_WIDTH + GLA_GATE_RANK
D_FF = 7 * D_MODEL // 2
N_EXPERTS = 8
TOP_K = 2
N_DENSE = (DEPTH + 1) // 2
N_MOE = DEPTH // 2
DN_ALPHA = (2.0 * DEPTH) ** 0.25
DN_BETA = (8.0 * DEPTH) ** -0.25
N_MOD = 6
MOD_STD = 0.1
LN_EPS = 1e-5
RMS_EPS = 1e-6

kernel_name = 'hymba_s5_gla_deepnorm_moe_trunk'


def _layer_norm(x, gain, bias):
    xf = x.astype(jnp.float32)
    mu = jnp.mean(xf, axis=-1, keepdims=True)
    var = jnp.mean(jnp.square(xf - mu), axis=-1, keepdims=True)
    return ((xf - mu) * lax.rsqrt(var + LN_EPS)).astype(x.dtype) * gain + bias


def _rms_norm(x, gain):
    xf = x.astype(jnp.float32)
    return xf * lax.rsqrt(jnp.mean(xf * xf, axis=-1, keepdims=True) + RMS_EPS) * gain.astype(jnp.float32)


def _linear_recurrence_op(e1, e2):
    a1, b1 = e1
    a2, b2 = e2
    return a1 * a2, a2 * b1 + b2


def _s5_group(u, lam_re, lam_im, log_dt, b_re, b_im, c_re, c_im, d_skip, w_glu, b_glu):
    bsz, seq, _ = u.shape
    n_chunks = seq // S5_CHUNK
    f32 = jnp.float32
    uf = u.astype(f32)
    lam = lax.complex(lam_re.astype(f32), lam_im.astype(f32))
    dt = jnp.exp(log_dt.astype(f32))[:, None]
    lam_bar = jnp.exp(lam * dt)
    b_mat = lax.complex(b_re.astype(f32), b_im.astype(f32))
    b_bar = ((lam_bar - 1.0) / lam)[:, :, None] * b_mat
    c_mat = lax.complex(c_re.astype(f32), c_im.astype(f32))
    u_chunks = uf.reshape(bsz, n_chunks, S5_CHUNK, S5_GROUPS, S5_GROUP_CH).transpose(1, 0, 2, 3, 4)
    a_elems = jnp.broadcast_to(lam_bar, (bsz, S5_CHUNK, S5_GROUPS, S5_STATE))

    def chunk_step(h, u_c):
        bu = jnp.einsum('gph,blgh->blgp', b_bar, u_c.astype(jnp.complex64))
        a_cum, s = lax.associative_scan(_linear_recurrence_op, (a_elems, bu), axis=1)
        states = s + a_cum * h[:, None]
        y = jnp.einsum('ghp,blgp->blgh', c_mat, states).real
        return states[:, -1], y

    h0 = jnp.zeros((bsz, S5_GROUPS, S5_STATE), jnp.complex64)
    _, y = lax.scan(chunk_step, h0, u_chunks)
    y = y.transpose(1, 0, 2, 3, 4).reshape(bsz, seq, S5_WIDTH) + d_skip.astype(f32) * uf
    y = jax.nn.gelu(y).astype(u.dtype)
    return y * jax.nn.sigmoid(y @ w_glu + b_glu)


def _gla_group(q, k, v, g_out, a_low, w_alpha_up, b_alpha, head_gain):
    bsz, seq, _ = q.shape
    nc = seq // GLA_CHUNK
    f32 = jnp.float32

    def heads(t, dh):
        return t.astype(f32).reshape(bsz, nc, GLA_CHUNK, GLA_HEADS, dh).transpose(1, 0, 3, 2, 4)

    log_alpha = jax.nn.log_sigmoid((a_low @ w_alpha_up + b_alpha).astype(f32)) / GLA_GATE_TAU
    qc = heads(q, GLA_HEAD_K) * (GLA_HEAD_K ** -0.5)
    kc = heads(k, GLA_HEAD_K)
    vc = heads(v, GLA_HEAD_V)
    bc = jnp.cumsum(heads(log_alpha, GLA_HEAD_K), axis=3)
    causal = jnp.tril(jnp.ones((GLA_CHUNK, GLA_CHUNK), dtype=bool))

    def chunk_step(state, inp):
        q_c, k_c, v_c, b_c = inp
        o_inter = jnp.einsum('bhik,bhkv->bhiv', q_c * jnp.exp(b_c), state)
        diff = b_c[:, :, :, None, :] - b_c[:, :, None, :, :]
        decay = jnp.exp(jnp.where(causal[:, :, None], diff, -jnp.inf))
        scores = jnp.einsum('bhik,bhjk,bhijk->bhij', q_c, k_c, decay)
        o_intra = jnp.einsum('bhij,bhjv->bhiv', scores, v_c)
        b_last = b_c[:, :, -1:, :]
        k_dec = k_c * jnp.exp(b_last - b_c)
        new_state = jnp.exp(b_last[:, :, 0, :])[..., None] * state + jnp.einsum('bhjk,bhjv->bhkv', k_dec, v_c)
        return new_state, o_inter + o_intra

    s0 = jnp.zeros((bsz, GLA_HEADS, GLA_HEAD_K, GLA_HEAD_V), f32)
    _, o = lax.scan(chunk_step, s0, (qc, kc, vc, bc))
    o = o.transpose(1, 0, 3, 2, 4).reshape(bsz, seq, GLA_HEADS, GLA_HEAD_V)
    o = _rms_norm(o, head_gain.reshape(GLA_HEADS, GLA_HEAD_V)).reshape(bsz, seq, GLA_WIDTH)
    return (o * jax.nn.silu(g_out.astype(f32))).astype(q.dtype)


def _swiglu(x, w_gate, w_up, w_down):
    return (jax.nn.silu(x @ w_gate) * (x @ w_up)) @ w_down


def _moe_swiglu(x, w_router, b_router, w_gate, w_up, w_down):
    logits = (x @ w_router).astype(jnp.float32) + b_router.astype(jnp.float32)
    top_vals, top_idx = lax.top_k(logits, TOP_K)
    top_w = jax.nn.softmax(top_vals, axis=-1)
    combine = jnp.sum(jax.nn.one_hot(top_idx, N_EXPERTS, dtype=jnp.float32) * top_w[..., None], axis=-2)
    out = jnp.zeros_like(x)
    for e in range(N_EXPERTS):
        out = out + combine[..., e:e + 1].astype(x.dtype) * _swiglu(x, w_gate[e], w_up[e], w_down[e])
    return out


def setup_inputs(seed: int = 0) -> dict:
    key = jax.random.key(seed)
    ks = jax.random.split(key, 32)
    f32 = jnp.float32

    def nrm(k, shape, std):
        return std * jax.random.normal(k, shape, f32)

    G, P, H = S5_GROUPS, S5_STATE, S5_GROUP_CH
    return {
        'x': jax.random.normal(ks[0], (BATCH, SEQ, D_MODEL), f32),
        'c': jax.random.normal(ks[1], (BATCH, D_MODEL), f32),
        'mod_w': nrm(ks[2], (DEPTH, D_MODEL, N_MOD * D_MODEL), MOD_STD * D_MODEL ** -0.5),
        'mod_b': nrm(ks[3], (DEPTH, N_MOD * D_MODEL), 0.01),
        'w_in': nrm(ks[4], (DEPTH, D_MODEL, IN_COLS), D_MODEL ** -0.5),
        'w_out': nrm(ks[5], (DEPTH, D_MIX, D_MODEL), DN_BETA * D_MIX ** -0.5),
        's5_lam_re': -0.5 * jnp.exp(nrm(ks[6], (DEPTH, G, P), 0.05)),
        's5_lam_im': math.pi * jnp.arange(P, dtype=f32)[None, None, :] + nrm(ks[7], (DEPTH, G, P), 0.01),
        's5_log_dt': jax.random.uniform(ks[8], (DEPTH, G), f32, math.log(1e-3), math.log(1e-1)),
        's5_b_re': nrm(ks[9], (DEPTH, G, P, H), (2.0 * H) ** -0.5),
        's5_b_im': nrm(ks[10], (DEPTH, G, P, H), (2.0 * H) ** -0.5),
        's5_c_re': nrm(ks[11], (DEPTH, G, H, P), (2.0 * P) ** -0.5),
        's5_c_im': nrm(ks[12], (DEPTH, G, H, P), (2.0 * P) ** -0.5),
        's5_d': nrm(ks[13], (DEPTH, S5_WIDTH), 1.0),
        's5_w_glu': nrm(ks[14], (DEPTH, S5_WIDTH, S5_WIDTH), S5_WIDTH ** -0.5),
        's5_b_glu': nrm(ks[15], (DEPTH, S5_WIDTH), 0.01),
        'gla_w_alpha_up': nrm(ks[16], (DEPTH, GLA_GATE_RANK, GLA_DK), GLA_GATE_RANK ** -0.5),
        'gla_b_alpha': nrm(ks[17], (DEPTH, GLA_DK), 0.01),
        'gla_head_gain': 1.0 + nrm(ks[18], (DEPTH, GLA_WIDTH), 0.02),
        'ln_mix_g': 1.0 + nrm(ks[19], (DEPTH, D_MODEL), 0.02),
        'ln_mix_b': nrm(ks[20], (DEPTH, D_MODEL), 0.01),
        'ffn_w_gate': nrm(ks[21], (N_DENSE, D_MODEL, D_FF), D_MODEL ** -0.5),
        'ffn_w_up': nrm(ks[22], (N_DENSE, D_MODEL, D_FF), D_MODEL ** -0.5),
        'ffn_w_down': nrm(ks[23], (N_DENSE, D_FF, D_MODEL), DN_BETA * D_FF ** -0.5),
        'moe_w_router': nrm(ks[24], (N_MOE, D_MODEL, N_EXPERTS), D_MODEL ** -0.5),
        'moe_b_router': nrm(ks[25], (N_MOE, N_EXPERTS), 0.01),
        'moe_w_gate': nrm(ks[26], (N_MOE, N_EXPERTS, D_MODEL, D_FF), D_MODEL ** -0.5),
        'moe_w_up': nrm(ks[27], (N_MOE, N_EXPERTS, D_MODEL, D_FF), D_MODEL ** -0.5),
        'moe_w_down': nrm(ks[28], (N_MOE, N_EXPERTS, D_FF, D_MODEL), DN_BETA * D_FF ** -0.5),
        'ln_ffn_g': 1.0 + nrm(ks[29], (DEPTH, D_MODEL), 0.02),
        'ln_ffn_b': nrm(ks[30], (DEPTH, D_MODEL), 0.01),
    }


def reference(x, c, mod_w, mod_b, w_in, w_out, s5_lam_re, s5_lam_im, s5_log_dt, s5_b_re, s5_b_im, s5_c_re, s5_c_im, s5_d, s5_w_glu, s5_b_glu, gla_w_alpha_up, gla_b_alpha, gla_head_gain, ln_mix_g, ln_mix_b, ffn_w_gate, ffn_w_up, ffn_w_down, moe_w_router, moe_b_router, moe_w_gate, moe_w_up, moe_w_down, ln_ffn_g, ln_ffn_b):
    c_act = jax.nn.silu(c)
    for layer in range(DEPTH):
        mod = c_act @ mod_w[layer] + mod_b[layer]
        sh_m, sc_m, gt_m, sh_f, sc_f, gt_f = jnp.split(mod[:, None, :], N_MOD, axis=-1)
        h = x * (1 + sc_m) + sh_m
        u, q, k, v, g_out, a_low = jnp.split(h @ w_in[layer], IN_SPLITS, axis=-1)
        y_s5 = _s5_group(u, s5_lam_re[layer], s5_lam_im[layer], s5_log_dt[layer], s5_b_re[layer], s5_b_im[layer], s5_c_re[layer], s5_c_im[layer], s5_d[layer], s5_w_glu[layer], s5_b_glu[layer])
        y_gla = _gla_group(q, k, v, g_out, a_low, gla_w_alpha_up[layer], gla_b_alpha[layer], gla_head_gain[layer])
        y = jnp.concatenate([y_s5, y_gla], axis=-1) @ w_out[layer]
        x = _layer_norm(DN_ALPHA * x + (1 + gt_m) * y, ln_mix_g[layer], ln_mix_b[layer])
        h = x * (1 + sc_f) + sh_f
        i = layer // 2
        if layer % 2 == 0:
            f = _swiglu(h, ffn_w_gate[i], ffn_w_up[i], ffn_w_down[i])
        else:
            f = _moe_swiglu(h, moe_w_router[i], moe_b_router[i], moe_w_gate[i], moe_w_up[i], moe_w_down[i])
        x = _layer_norm(DN_ALPHA * x + (1 + gt_f) * f, ln_ffn_g[layer], ln_ffn_b[layer])
    return x
```

```python
import math
import os
from contextlib import ExitStack

import numpy as np
import concourse.bass as bass
import concourse.mybir as mybir
from concourse.bass_utils import run_bass_kernel_spmd

F32 = mybir.dt.float32
BF16 = mybir.dt.bfloat16
I32 = mybir.dt.int32
AF = mybir.ActivationFunctionType
ALU = mybir.AluOpType

D = 1024
SEQ = 2048
NB = 32
DEPTH = 2
NCORES = 8
SPC = NB // NCORES
INC = 2064
DFF = 3584
NE = 8
NMOD = 6
ALPHA = (2.0 * DEPTH) ** 0.25
LN_EPS = 1e-5
RMS_EPS = 1e-6
TWO_PI = 2.0 * math.pi

ENGS = ['pe', 'act', 'dve', 'pool', 'sp']
SEM_EPOCH = 12000
DMA_SEMS = dict(sp=10, pool=14, act=4, dve=2, pe=2)
SAME_ENGINE_SYNC = True
ROT_ENG = os.environ.get("MK_ROT_ENG", "dve")
LN_ENG = os.environ.get("MK_LN_ENG", "pool")

DBG = os.environ.get("MK_DEBUG", "")
N_SEQ = int(os.environ.get("MK_NSEQ", str(SPC)))
N_LAYER = int(os.environ.get("MK_NLAYER", str(DEPTH)))
STOP_AFTER = os.environ.get("MK_STOP", "")


class Buf:
    __slots__ = ('name', 'writers', 'readers')

    def __init__(self, name=''):
        self.name = name
        self.writers = {}
        self.readers = {}


def _merge(dst, src):
    for k, v in src.items():
        if dst.get(k, -1) < v:
            dst[k] = v


class Prog:
    def __init__(self, nc):
        self.nc = nc
        self.stack = ExitStack()
        self.sems = []
        self.eng = dict(pe=nc.tensor, act=nc.scalar, dve=nc.vector, pool=nc.gpsimd, sp=nc.sync)
        self.esem = {e: None for e in ENGS}
        self.known = {e: {} for e in ENGS}
        self.pending = {e: ([], []) for e in ENGS}
        self.dsem = {}
        self.ninst = {e: 0 for e in ENGS}
        self.all_dma = {}

    def new_sem(self):
        i = len(self.sems)
        self.sems.append(self.stack.enter_context(self.nc.semaphore(f"s{i}")))
        return i

    def tile(self, name, shape, dt):
        return self.stack.enter_context(self.nc.sbuf_tensor(name, list(shape), dt))

    def psum(self, name, shape, dt=F32):
        return self.stack.enter_context(self.nc.psum_tensor(name, list(shape), dt))

    def _wait(self, e, deps):
        eng = self.eng[e]
        kn = self.known[e]
        for (semid, val) in deps.items():
            if kn.get(semid, 0) >= val:
                continue
            eng.wait_ge(self.sems[semid], val)
            kn[semid] = val
            self.ninst[e] += 1

    def _collect(self, reads, writes):
        deps = {}
        for b in reads:
            _merge(deps, b.writers)
        for b in writes:
            _merge(deps, b.writers)
            _merge(deps, b.readers)
        return deps

    def _next_sig(self, e):
        cur = self.esem[e]
        if cur is None or cur[1] >= SEM_EPOCH:
            cur = (self.new_sem(), 0)
        cur = (cur[0], cur[1] + 1)
        self.esem[e] = cur
        return cur

    def op(self, e, fn, reads=(), writes=(), sig=True):
        deps = self._collect(reads, writes)
        if e == 'pe' or not SAME_ENGINE_SYNC:
            cur = self.esem[e]
            if cur is not None:
                deps.pop(cur[0], None)
        self._wait(e, deps)
        ins = fn(self.eng[e])
        self.ninst[e] += 1
        pr, pw = self.pending[e]
        if not sig:
            pr.extend(reads)
            pw.extend(writes)
            return ins
        semid, val = self._next_sig(e)
        ins.then_inc(self.sems[semid], 1)
        me = {semid: val}
        for b in list(pr) + list(reads):
            _merge(b.readers, me)
        for b in list(pw) + list(writes):
            b.writers = dict(me)
            b.readers = {}
        pr.clear()
        pw.clear()
        return ins

    def dma(self, q, out, in_, reads=(), writes=(), **kw):
        deps = self._collect(reads, writes)
        st = self.dsem.get(q)
        if st is None:
            k = DMA_SEMS[q]
            st = dict(sems=[self.new_sem() for _ in range(k)], vals=[0] * k, rr=0)
            self.dsem[q] = st
        j = st['rr'] % len(st['sems'])
        st['rr'] += 1
        semid = st['sems'][j]
        prev = st['vals'][j]
        if prev > 0 and deps.get(semid, 0) < prev:
            deps[semid] = prev
        self._wait(q, deps)
        val = prev + 16
        st['vals'][j] = val
        kw.setdefault('allow_slow_non_contiguous', True)
        self.eng[q].dma_start(out=out, in_=in_, **kw).then_inc(self.sems[semid], 16)
        self.ninst[q] += 1
        me = {semid: val}
        _merge(self.all_dma, me)
        for b in reads:
            _merge(b.readers, me)
        for b in writes:
            b.writers = dict(me)
            b.readers = {}

    def barrier(self, engines=ENGS):
        deps = dict(self.all_dma)
        for e in ENGS:
            cur = self.esem[e]
            if cur is not None:
                deps[cur[0]] = cur[1]
        for e in engines:
            self._wait(e, dict(deps))

    def finish(self):
        self.barrier(['sp'])
        self.stack.close()


class Arena:
    def __init__(self, tile, n):
        self.t = tile
        self.n = n
        self.off = 0

    def reset(self):
        self.off = 0

    def f32(self, shape, parts=128):
        n = int(np.prod(shape))
        assert self.off + n <= self.n, ("arena overflow", self.off, n, self.n)
        v = self.t[0:parts, self.off:self.off + n]
        self.off += n
        return self._shape(v, shape)

    def bf16(self, shape, parts=128):
        n = int(np.prod(shape))
        n32 = (n + 1) // 2
        assert self.off + n32 <= self.n, ("arena overflow", self.off, n32, self.n)
        v = self.t[0:parts, self.off:self.off + n32].bitcast(BF16)
        if n32 * 2 != n:
            v = v[:, 0:n]
        self.off += n32
        return self._shape(v, shape)

    def i32(self, shape, parts=128):
        n = int(np.prod(shape))
        v = self.t[0:parts, self.off:self.off + n].bitcast(I32)
        self.off += n
        return self._shape(v, shape)

    @staticmethod
    def _shape(v, shape):
        if len(shape) == 1:
            return v
        if len(shape) == 2:
            return v.rearrange("p (a b) -> p a b", a=shape[0])
        if len(shape) == 3:
            return v.rearrange("p (a b c) -> p a b c", a=shape[0], b=shape[1])
        if len(shape) == 4:
            return v.rearrange("p (a b c d) -> p a b c d", a=shape[0], b=shape[1], c=shape[2])
        raise ValueError(shape)


def build_program():
    nc = bass.Bass("TRN2", target_bir_lowering=False)
    P = Prog(nc)

    def din(name, shape, dt=F32):
        return nc.dram_tensor(name, list(shape), dt, kind="ExternalInput").ap()

    x_d = din("x", [SPC, SEQ, D])
    cT_d = din("cT", [128, 8, SPC])
    mod_w = din("mod_w", [DEPTH, D, NMOD * D])
    mod_b = din("mod_b", [DEPTH, NMOD * D])
    w_in = din("w_in", [DEPTH, D, INC])
    w_out = din("w_out", [DEPTH, D, D])
    lam_re = din("s5_lam_re", [DEPTH, 2048])
    lam_im = din("s5_lam_im", [DEPTH, 2048])
    log_dt = din("s5_log_dt", [DEPTH, 32])
    b_re = din("s5_b_re", [DEPTH, 32 * 64 * 16])
    b_im = din("s5_b_im", [DEPTH, 32 * 64 * 16])
    c_re = din("s5_c_re", [DEPTH, 32, 16, 64])
    c_im = din("s5_c_im", [DEPTH, 32, 16, 64])
    s5_d = din("s5_d", [DEPTH, 512])
    w_glu = din("s5_w_glu", [DEPTH, 512, 512])
    b_glu = din("s5_b_glu", [DEPTH, 512])
    w_aup = din("gla_w_alpha_up", [DEPTH, 16, 256])
    b_alpha = din("gla_b_alpha", [DEPTH, 256])
    head_gain = din("gla_head_gain", [DEPTH, 512])
    ln_mix_g = din("ln_mix_g", [DEPTH, D])
    ln_mix_b = din("ln_mix_b", [DEPTH, D])
    ffn_wg = din("ffn_w_gate", [1, 1, D, DFF])
    ffn_wu = din("ffn_w_up", [1, 1, D, DFF])
    ffn_wd = din("ffn_w_down", [1, 1, DFF, D])
    w_router = din("moe_w_router", [1, D, NE])
    b_router = din("moe_b_router", [1, NE])
    moe_wg = din("moe_w_gate", [1, NE, D, DFF])
    moe_wu = din("moe_w_up", [1, NE, D, DFF])
    moe_wd = din("moe_w_down", [1, NE, DFF, D])
    ln_ffn_g = din("ln_ffn_g", [DEPTH, D])
    ln_ffn_b = din("ln_ffn_b", [DEPTH, D])
    ident_d = din("k_ident", [128, 128])
    mask_d = din("k_mask", [128, 128])
    m0_d = din("k_m0", [128, 512])
    tau_d = din("k_tau", [128, 128])
    out_d = nc.dram_tensor("out", [SPC, SEQ, D], F32, kind="ExternalOutput").ap()
    gt_scr = nc.dram_tensor("gt_scr", [DEPTH, 2, SPC, D], F32, kind="Internal").ap()
    dbg_outs = {}

    def dbg_dump(name, ap_sb, shape, buf, dt=F32):
        if not DBG:
            return
        d = nc.dram_tensor("dbg_" + name, list(shape), dt, kind="ExternalOutput").ap()
        dbg_outs[name] = d
        P.dma('sp', d, ap_sb, reads=[buf], writes=[Buf()])

    x_sb = P.tile("x_sb", [128, 16, D], F32)
    bx = [Buf(f"x{t}") for t in range(16)]
    ARENA_N = 30848
    arena_t = P.tile("arena", [128, ARENA_N], F32)
    A = Arena(arena_t, ARENA_N)
    ident = P.tile("ident", [128, 128], F32)
    identb = P.tile("identb", [128, 128], BF16)
    mask128 = P.tile("mask128", [128, 128], F32)
    m0tab = P.tile("m0tab", [128, 512], F32)
    tau1 = P.tile("tau1", [128, 128], F32)
    onesdiv = P.tile("onesdiv", [128, 128], F32)
    ones_row = P.tile("ones_row", [1, 128], F32)
    eps_ln = P.tile("eps_ln", [128, 1], F32)
    eps_rms = P.tile("eps_rms", [128, 1], F32)
    cact = P.tile("cact", [128, 8, SPC], F32)
    modT = P.tile("modT", [128, DEPTH, 48, SPC], F32)
    gt_bc = P.tile("gt_bc", [128, D], F32)
    g_bc = P.tile("g_bc", [128, D], F32)
    b_bc = P.tile("b_bc", [128, D], F32)
    small = P.tile("small", [128, 256], F32)
    bconst = Buf("const")
    bmod = Buf("mod")
    bgt, bgb, bbb = Buf("gt_bc"), Buf("g_bc"), Buf("b_bc")
    bsmall = Buf("small")

    pst = [P.psum(f"ps{i}", [128, 1024]) for i in range(4)]
    PB = [pst[i // 2][:, (i % 2) * 512:(i % 2) * 512 + 512] for i in range(8)]
    bPB = [Buf(f"bank{i}") for i in range(8)]

    def mm(out, lhsT, rhs, start, stop, reads, writes, sig=None, **kw):
        if sig is None:
            sig = stop
        return P.op('pe', lambda e: e.matmul(out, lhsT=lhsT, rhs=rhs, start=start, stop=stop, **kw),
                    reads=reads, writes=writes, sig=sig)

    def tr(out, in_, idt, reads, writes, sig=True):
        return P.op('pe', lambda e: e.transpose(out=out, in_=in_, identity=idt), reads=reads, writes=writes, sig=sig)

    def act(out, in_, func, reads, writes, scale=None, bias=None):
        kw = {}
        if scale is not None:
            kw['scale'] = scale
        if bias is not None:
            kw['bias'] = bias
        return P.op('act', lambda e: e.activation(out=out, in_=in_, func=func, **kw), reads=reads, writes=writes)

    def tt(out, in0, in1, op, reads, writes, eng='dve'):
        return P.op(eng, lambda e: e.tensor_tensor(out=out, in0=in0, in1=in1, op=op), reads=reads, writes=writes)

    def ts(out, in0, s1, op0, reads, writes, s2=None, op1=None, eng='dve'):
        if op1 is None:
            return P.op(eng, lambda e: e.tensor_scalar(out=out, in0=in0, scalar1=s1, scalar2=None, op0=op0),
                        reads=reads, writes=writes)
        return P.op(eng, lambda e: e.tensor_scalar(out=out, in0=in0, scalar1=s1, scalar2=s2, op0=op0, op1=op1),
                    reads=reads, writes=writes)

    def stt(out, in0, scalar, in1, op0, op1, reads, writes):
        return P.op('dve', lambda e: e.scalar_tensor_tensor(out=out, in0=in0, scalar=scalar, in1=in1, op0=op0, op1=op1),
                    reads=reads, writes=writes)

    def copy(out, in_, reads, writes, eng='dve'):
        if eng == 'act':
            return act(out, in_, AF.Copy, reads, writes)
        return P.op(eng, lambda e: e.tensor_copy(out=out, in_=in_), reads=reads, writes=writes)

    def memset(out, val, writes, eng='dve'):
        return P.op(eng, lambda e: e.memset(out, val), reads=(), writes=writes)

    def load_x(s_):
        for t in range(16):
            P.dma('sp', x_sb[:, t, :], x_d[s_, t * 128:(t + 1) * 128, :], writes=[bx[t]])

    load_x(0)
    P.dma('sp', ident[:], ident_d, writes=[bconst])
    P.dma('sp', mask128[:], mask_d, writes=[bconst])
    P.dma('sp', m0tab[:], m0_d, writes=[bconst])
    P.dma('sp', tau1[:], tau_d, writes=[bconst])
    copy(identb[:], ident[:], [bconst], [bconst])
    memset(onesdiv[:], 1.0 / 128.0, [bconst])
    memset(ones_row[:], 1.0, [bconst])
    memset(eps_ln[:], LN_EPS, [bconst])
    memset(eps_rms[:], RMS_EPS, [bconst])
    A.reset()
    ctmp = A.f32([8, SPC])
    bct = Buf()
    P.dma('sp', ctmp, cT_d, writes=[bct])
    act(cact[:], ctmp, AF.Silu, [bct], [bconst])

    mb = A.f32([NMOD * D], parts=1)
    mw = [A.f32([8, 512]) for _ in range(2)]
    gtr = [A.f32([512], parts=SPC) for _ in range(2)]
    bmb = Buf()
    bmw = [Buf(), Buf()]
    bgtr = [Buf(), Buf()]
    it = 0
    for l in range(DEPTH):
        P.dma('sp', mb, mod_b[l:l + 1, :], writes=[bmb])
        for cb in range(12):
            bi = it % 2
            it += 1
            which = cb // 2
            P.dma('sp', mw[bi], mod_w[l, :, cb * 512:(cb + 1) * 512].rearrange("(k p) n -> p k n", p=128),
                  writes=[bmw[bi]])
            pb = PB[it % 2]
            bpb = bPB[it % 2]
            for cc in range(4):
                for k in range(8):
                    mm(pb[:, cc * 4:cc * 4 + 4], mw[bi][:, k, cc * 128:(cc + 1) * 128], cact[:, k, :],
                       k == 0, False, [bmw[bi], bconst], [bpb], sig=False)
                mm(pb[:, cc * 4:cc * 4 + 4], mb[0:1, cb * 512 + cc * 128:cb * 512 + (cc + 1) * 128],
                   ones_row[0:1, 0:SPC], False, True, [bmb, bconst], [bpb], sig=True)
            dst = modT[:, l, cb * 4:(cb + 1) * 4, :]
            src = pb[:, 0:16].rearrange("p (a b) -> p a b", a=4)
            if which in (1, 2, 4, 5):
                ts(dst, src, 1.0, ALU.add, [bpb], [bmod])
            else:
                copy(dst, src, [bpb], [bmod])
            if which in (2, 5):
                pr_ = PB[2 + it % 2]
                bpr = bPB[2 + it % 2]
                for k in range(8):
                    mm(pr_[0:SPC, :], cact[:, k, :], mw[bi][:, k, :], k == 0, False, [bmw[bi], bconst], [bpr], sig=False)
                mm(pr_[0:SPC, :], ones_row[0:1, 0:SPC], mb[0:1, cb * 512:(cb + 1) * 512], False, True,
                   [bmb, bconst], [bpr], sig=True)
                ts(gtr[bi], pr_[0:SPC, :], 1.0, ALU.add, [bpr], [bgtr[bi]])
                wsel = 0 if which == 2 else 1
                half = cb % 2
                P.dma('sp', gt_scr[l, wsel, :, half * 512:(half + 1) * 512], gtr[bi], reads=[bgtr[bi]], writes=[bgt])
    P.barrier()

    def load_bcast(dst, bdst, src_vec):
        P.dma('sp', dst[:], src_vec.partition_broadcast(128), writes=[bdst])

    def make_hT(l, which_sc, which_sh, tt_, dst, bdst, f32_copy=None, bf32=None):
        for kc in range(8):
            pb = PB[kc % 2]
            bpb = bPB[kc % 2]
            for sub in range(4):
                t = tt_ * 4 + sub
                tr(pb[:, sub * 128:(sub + 1) * 128], x_sb[:, t, kc * 128:(kc + 1) * 128], ident[:],
                   [bx[t], bconst], [bpb], sig=(sub == 3))
            sc = modT[:, l, which_sc * 8 + kc, cur_s[0]:cur_s[0] + 1]
            sh = modT[:, l, which_sh * 8 + kc, cur_s[0]:cur_s[0] + 1]
            act(dst[:, kc, :], pb, AF.Identity, [bpb, bmod], [bdst], scale=sc, bias=sh)
            if f32_copy is not None:
                act(f32_copy[:, kc, :], pb, AF.Identity, [bpb, bmod], [bf32], scale=sc, bias=sh)

    cur_s = [0]

    def layer_norm_tile(t, reads_extra=()):
        st = lnw_stats[t % 2]
        bst = blnw[t % 2]
        P.op('dve', lambda e: e.bn_stats(out=st[:, 0:6], in_=x_sb[:, t, 0:512]), reads=[bx[t]], writes=[bst])
        P.op('dve', lambda e: e.bn_stats(out=st[:, 6:12], in_=x_sb[:, t, 512:1024]), reads=[bx[t]], writes=[bst])
        P.op('dve', lambda e: e.bn_aggr(out=st[:, 12:14], in_=st[:, 0:12]), reads=[bst], writes=[bst])
        act(st[:, 14:15], st[:, 13:14], AF.Sqrt, [bst], [bst], bias=eps_ln[:])
        P.op('dve', lambda e: e.reciprocal(out=st[:, 15:16], in_=st[:, 14:15]), reads=[bst], writes=[bst])
        stt(st[:, 16:17], st[:, 12:13], -1.0, st[:, 15:16], ALU.mult, ALU.mult, [bst], [bst])
        act(x_sb[:, t, :], x_sb[:, t, :], AF.Identity, [bst, bx[t]], [bx[t]], scale=st[:, 15:16], bias=st[:, 16:17])
        tt(x_sb[:, t, :], x_sb[:, t, :], g_bc[:], ALU.mult, [bx[t], bgb], [bx[t]], eng=LN_ENG)
        tt(x_sb[:, t, :], x_sb[:, t, :], b_bc[:], ALU.add, [bx[t], bbb], [bx[t]], eng=LN_ENG)

    lnw_t = P.tile("lnw", [128, 2, 32], F32)
    lnw_stats = [lnw_t[:, 0, :], lnw_t[:, 1, :]]
    blnw = [Buf(), Buf()]

    def pass_a(l, s):
        A.reset()
        uT = A.bf16([4, SEQ])
        buT = [Buf() for _ in range(4)]
        win = A.bf16([8, INC])
        w_in_v = w_in[l].rearrange("(k p) n -> p k n", p=128)
        bwin_g = {}
        for (c0, c1) in ((0, 512), (1024, 1536), (2048, INC), (512, 1024), (1536, 2048)):
            bg_ = Buf()
            P.dma('pool', win[:, :, c0:c1], w_in_v[:, :, c0:c1], writes=[bg_])
            bwin_g[c0] = bg_

        def bwin_of(col):
            if col < 512:
                return bwin_g[0]
            if col < 1024:
                return bwin_g[512]
            if col < 1536:
                return bwin_g[1024]
            if col < 2048:
                return bwin_g[1536]
            return bwin_g[2048]
        wo = A.bf16([4, D])
        bwo = Buf()
        P.dma('pool', wo, w_out[l, 512:1024, :].rearrange("(k p) n -> p k n", p=128), writes=[bwo])
        hTt = A.bf16([8, 512])
        bhTt = Buf()
        waup = A.bf16([256], parts=16)
        bwa = Buf()
        P.dma('pool', waup, w_aup[l], writes=[bwa])
        P.dma('sp', small[:, 0:2], b_alpha[l].rearrange("(h q) -> q h", q=128), writes=[bsmall],
              allow_slow_non_contiguous=True)
        P.dma('sp', small[:, 2:6], head_gain[l].rearrange("(h q) -> q h", q=128), writes=[bsmall],
              allow_slow_non_contiguous=True)
        ts(small[:, 0:2], small[:, 0:2], -1.0, ALU.mult, [bsmall], [bsmall])
        P.dma('sp', gt_bc[:], gt_scr[l, 0, s].partition_broadcast(128), writes=[bgt])
        vtok = A.bf16([4, 512])
        bvt = Buf()
        a16 = A.bf16([512], parts=16)
        ba16 = Buf()
        lsp = A.f32([2, 512])
        blsp = Buf()
        bcum = A.f32([2, 512])
        bbc = Buf()
        eb = A.f32([2, 512])
        beb = Buf()
        enb = A.f32([2, 512])
        benb = Buf()
        qT = A.bf16([2, 512])
        bqT = Buf()
        kz = [A.bf16([2, 512]) for _ in range(2)]
        bkz = [Buf(), Buf()]
        ktok = A.bf16([2, 2, 4, 128])
        bkt = Buf()
        ssb = A.bf16([2, 2, 4, 128])
        bss = [Buf(), Buf()]
        state = A.f32([4, 128])
        bstt = [Buf() for _ in range(4)]
        stbf = A.bf16([4, 128])
        bsbf = [Buf() for _ in range(4)]
        tmpT = A.f32([4, 128])
        btmp = [Buf() for _ in range(4)]
        sq = bcum[:, 0, :]
        rstd = bcum[:, 1, :]
        bsq = bbc
        brs = bbc
        t1 = enb[:, 0, :]
        sg = enb[:, 1, :]
        bt1 = benb
        bsg = benb
        ygla = A.bf16([4, 512])
        byg = Buf()
        tw = lsp.rearrange("p a b -> p (a b)")
        btw = blsp
        memset(kz[0], 0.0, [bkz[0]])
        memset(kz[1], 0.0, [bkz[1]])
        memset(state, 0.0, bstt)
        memset(stbf, 0.0, bsbf)
        for tt_ in range(4):
            make_hT(l, 1, 0, tt_, hTt, bhTt)
            hs = slice(tt_ * 512, (tt_ + 1) * 512)
            rot = [0]

            def proj(col0, m, evac):
                bi = 2 + rot[0] % 2
                rot[0] += 1
                for k in range(8):
                    mm(PB[bi][0:m, :], win[:, k, col0:col0 + m], hTt[:, k, :], k == 0, k == 7,
                       [bwin_of(col0), bhTt], [bPB[bi]])
                evac(PB[bi][0:m, :], bPB[bi])

            for c4 in range(4):
                proj(c4 * 128, 128, lambda ps, b, c4=c4: copy(uT[:, c4, hs], ps, [b], [buT[tt_]], eng='act'))
            for sub in range(4):
                bi = 2 + rot[0] % 2
                rot[0] += 1
                for k in range(8):
                    mm(PB[bi], hTt[:, k, sub * 128:(sub + 1) * 128], win[:, k, 1024:1536],
                       k == 0, k == 7, [bwin_of(1024), bhTt], [bPB[bi]])
                copy(vtok[:, sub, :], PB[bi], [bPB[bi]], [bvt], eng='act')
            proj(2048, 16, lambda ps, b: copy(a16, ps, [b], [ba16], eng='act'))
            for hp in range(2):
                bi = 2 + rot[0] % 2
                rot[0] += 1
                mm(PB[bi], waup[:, hp * 128:(hp + 1) * 128], a16, True, True, [bwa, ba16], [bPB[bi]])
                act(lsp[:, hp, :], PB[bi], AF.Exp, [bPB[bi], bsmall], [blsp], scale=-1.0, bias=small[:, hp:hp + 1])
            act(lsp, lsp, AF.Ln, [blsp], [blsp], bias=1.0)
            for hp in range(2):
                P.op('dve', lambda e, hp=hp: e.tensor_tensor_scan(out=bcum[:, hp, :], data0=m0tab[:], data1=lsp[:, hp, :],
                                                                  initial=0.0, op0=ALU.mult, op1=ALU.add),
                     reads=[blsp, bconst], writes=[bbc])
            act(eb, bcum, AF.Exp, [bbc], [beb], scale=-1.0 / 16.0)
            act(enb, bcum, AF.Exp, [bbc], [benb], scale=1.0 / 16.0)
            for hp in range(2):
                proj(512 + hp * 128, 128,
                     lambda ps, b, hp=hp: stt(qT[:, hp, :], ps, 0.125, eb[:, hp, :], ALU.mult, ALU.mult, [b, beb], [bqT]))
            for hp in range(2):
                def ev(ps, b, hp=hp):
                    for m in range(2):
                        tt(kz[m][64 * m:64 * m + 64, hp, :], ps[64 * m:64 * m + 64, :], enb[64 * m:64 * m + 64, hp, :],
                           ALU.mult, [b, benb], [bkz[m]])
                proj(768 + hp * 128, 128, ev)
            for m in range(2):
                pbb = PB[m].bitcast(BF16)
                for hp in range(2):
                    for sub in range(4):
                        idx = hp * 4 + sub
                        tr(pbb[:, idx * 128:(idx + 1) * 128], kz[m][:, hp, sub * 128:(sub + 1) * 128], identb[:],
                           [bkz[m], bconst], [bPB[m]], sig=(idx == 7))
                copy(ktok[:, m, :, :, :].rearrange("p a b c -> p (a b c)"), pbb, [bPB[m]], [bkt], eng='act')
            for hp in range(2):
                for m in range(2):
                    bi = 4 + m
                    for sub in range(4):
                        cs = slice(sub * 128, (sub + 1) * 128)
                        mm(PB[bi][:, cs], kz[m][:, hp, cs], qT[:, hp, cs], True, True, [bkz[m], bqT], [bPB[bi]],
                           sig=(sub == 3))
                    tt(ssb[:, hp, m, :, :], PB[bi].rearrange("p (a b) -> p a b", a=4),
                       mask128[:].unsqueeze(1).broadcast_to([128, 4, 128]), ALU.mult, [bPB[bi], bconst], [bss[hp]])
            for sub in range(4):
                cs = slice(sub * 128, (sub + 1) * 128)
                for h in range(4):
                    hp, m = h // 2, h % 2
                    mm(PB[h][:, cs], vtok[:, sub, h * 128:(h + 1) * 128], ssb[:, hp, m, sub, :], True, False,
                       [bvt, bss[hp]], [bPB[h]], sig=False)
                    mm(PB[h][:, cs], stbf[:, h, :], qT[:, hp, cs], False, True, [bsbf[h], bqT], [bPB[h]], sig=True)
                for h in range(4):
                    hp, m = h // 2, h % 2
                    ub = 6 + (h % 2)
                    mm(PB[ub][:, 0:128], ktok[:, m, hp, sub, :], vtok[:, sub, h * 128:(h + 1) * 128], True, True,
                       [bkt, bvt], [bPB[ub]])
                    tt(tmpT[:, h, :], PB[ub][:, 0:128], state[:, h, :], ALU.add, [bPB[ub], bstt[h]], [btmp[h]])
                    ts(state[:, h, :], tmpT[:, h, :], eb[:, hp, sub * 128 + 127: sub * 128 + 128], ALU.mult,
                       [btmp[h], beb], [bstt[h]])
                    copy(stbf[:, h, :], state[:, h, :], [bstt[h]], [bsbf[h]], eng='act')
            for h in range(4):
                act(sq, PB[h], AF.Square, [bPB[h]], [bsq])
                mb_ = 4 + h % 2
                mm(PB[mb_], onesdiv[:], sq, True, True, [bconst, bsq], [bPB[mb_]])
                act(rstd, PB[mb_], AF.Sqrt, [bPB[mb_], bconst], [brs], bias=eps_rms[:])
                P.op('dve', lambda e: e.reciprocal(out=rstd, in_=rstd), reads=[brs], writes=[brs])
                stt(t1, PB[h], small[:, 2 + h:3 + h], rstd, ALU.mult, ALU.mult, [bPB[h], bsmall, brs], [bt1])
                gb = 6 + h % 2
                for k in range(8):
                    mm(PB[gb], win[:, k, 1536 + h * 128:1536 + (h + 1) * 128], hTt[:, k, :], k == 0, k == 7,
                       [bwin_of(1536), bhTt], [bPB[gb]])
                act(sg, PB[gb], AF.Silu, [bPB[gb]], [bsg])
                tt(ygla[:, h, :], t1, sg, ALU.mult, [bt1, bsg], [byg])
            if DBG and tt_ == 0 and l == 0 and s == 0:
                dbg_dump("ygla", ygla, [128, 4, 512], byg, BF16)
                dbg_dump("uT", uT[:, :, 0:512], [128, 4, 512], buT[0], BF16)
            for sub in range(4):
                t = tt_ * 4 + sub
                pp = 2 + sub % 2
                for half in range(2):
                    bi = pp * 2 + half
                    for h in range(4):
                        mm(PB[bi], ygla[:, h, sub * 128:(sub + 1) * 128], wo[:, h, half * 512:(half + 1) * 512],
                           h == 0, h == 3, [byg, bwo], [bPB[bi]])
                tt(tw, pst[pp][:, :], gt_bc[:], ALU.mult, [bPB[pp * 2], bPB[pp * 2 + 1], bgt], [btw])
                stt(x_sb[:, t, :], x_sb[:, t, :], ALPHA, tw, ALU.mult, ALU.add, [bx[t], btw], [bx[t]])
        return uT, buT

    def pass_b(l, s, uT, buT):
        A.reset()
        A.bf16([4, SEQ])
        wo = A.bf16([4, D])
        bwo = Buf()
        P.dma('pool', wo, w_out[l, 0:512, :].rearrange("(k p) n -> p k n", p=128), writes=[bwo])
        wgl = A.bf16([4, 512])
        bwgl = Buf()
        P.dma('pool', wgl, w_glu[l].rearrange("(k p) n -> p k n", p=128), writes=[bwgl])
        load_bcast(g_bc, bgb, ln_mix_g[l])
        load_bcast(b_bc, bbb, ln_mix_b[l])
        P.dma('sp', small[:, 8:12], s5_d[l].rearrange("(c q) -> q c", q=128), writes=[bsmall],
              allow_slow_non_contiguous=True)
        P.dma('sp', small[:, 12:16], b_glu[l].rearrange("(c q) -> q c", q=128), writes=[bsmall],
              allow_slow_non_contiguous=True)
        lre = A.f32([16])
        lim = A.f32([16])
        ldt = A.f32([16])
        bl = Buf()
        P.dma('sp', lre, lam_re[l].rearrange("(j q) -> q j", q=128), writes=[bl], allow_slow_non_contiguous=True)
        P.dma('sp', lim, lam_im[l].rearrange("(j q) -> q j", q=128), writes=[bl], allow_slow_non_contiguous=True)
        for g2 in range(2):
            P.dma('sp', ldt[64 * g2:64 * g2 + 64, :],
                  log_dt[l].rearrange("(j g) -> g j", g=2)[g2].partition_broadcast(64), writes=[bl])
        dtt = A.f32([16])
        th = A.f32([16])
        rr = A.f32([16])
        w1 = A.f32([16])
        w2 = A.f32([16])
        w3 = A.f32([16])
        cre = A.f32([16])
        cim = A.f32([16])
        bw = Buf()
        act(dtt, ldt, AF.Exp, [bl], [bw])
        tt(th, lim, dtt, ALU.mult, [bl, bw], [bw])
        tt(w1, lre, dtt, ALU.mult, [bl, bw], [bw])
        act(rr, w1, AF.Exp, [bw], [bw])
        ctab = A.f32([16, 128])
        stab = A.f32([16, 128])
        nre = A.f32([16])
        nim = A.f32([16])
        Bre_l = A.bf16([16, 128])
        Bim_l = A.bf16([16, 128])
        Cre_l = A.bf16([16, 32])
        CreN_l = A.bf16([16, 32])
        CimN_l = A.bf16([16, 32])
        blhs = Buf()
        scr0 = A.off
        ang = A.f32([16, 128])
        kf = A.f32([16, 128])
        ki = A.i32([16, 128])
        btab = Buf()
        bang = Buf()
        tt(ang, th.unsqueeze(2).broadcast_to([128, 16, 128]), tau1[:].unsqueeze(1).broadcast_to([128, 16, 128]),
           ALU.mult, [bw, bconst], [bang])

        def sin_of(dst, shift):
            ts(kf, ang, 1.0 / TWO_PI, ALU.mult, [bang], [bang], s2=shift / TWO_PI, op1=ALU.add)
            copy(ki, kf, [bang], [bang])
            copy(kf, ki, [bang], [bang])
            stt(kf, kf, -TWO_PI, ang, ALU.mult, ALU.add, [bang], [bang])
            ts(kf, kf, shift, ALU.add, [bang], [bang], s2=math.pi, op1=ALU.min)
            ts(kf, kf, -math.pi, ALU.max, [bang], [bang])
            act(dst, kf, AF.Sin, [bang], [btab])

        sin_of(stab, 0.0)
        sin_of(ctab, math.pi / 2.0)
        c0 = ctab[:, :, 0]
        s0 = stab[:, :, 0]
        tt(nre, rr, c0, ALU.mult, [bw, btab], [bw])
        ts(nre, nre, -1.0, ALU.add, [bw], [bw])
        tt(nim, rr, s0, ALU.mult, [bw, btab], [bw])
        tt(w1, lre, lre, ALU.mult, [bl], [bw])
        tt(w2, lim, lim, ALU.mult, [bl], [bw])
        tt(w1, w1, w2, ALU.add, [bw], [bw])
        P.op('dve', lambda e: e.reciprocal(out=w1, in_=w1), reads=[bw], writes=[bw])
        tt(w2, nre, lre, ALU.mult, [bw, bl], [bw])
        tt(w3, nim, lim, ALU.mult, [bw, bl], [bw])
        tt(w2, w2, w3, ALU.add, [bw], [bw])
        tt(cre, w2, w1, ALU.mult, [bw], [bw])
        tt(w2, nim, lre, ALU.mult, [bw, bl], [bw])
        tt(w3, nre, lim, ALU.mult, [bw, bl], [bw])
        tt(w2, w2, w3, ALU.subtract, [bw], [bw])
        tt(cim, w2, w1, ALU.mult, [bw], [bw])
        braw = A.f32([16, 16])
        biraw = A.f32([16, 16])
        bbr = A.f32([16, 16])
        bbi = A.f32([16, 16])
        p1 = A.f32([16, 16])
        Wz = A.f32([16, 128])
        Vz = A.f32([16, 128], parts=32)
        bB = Buf()
        bWz = Buf()
        bVz = Buf()
        P.dma('sp', braw, b_re[l].rearrange("(j q h) -> q j h", q=128, h=16), writes=[bB])
        P.dma('sp', biraw, b_im[l].rearrange("(j q h) -> q j h", q=128, h=16), writes=[bB])
        creb = cre.unsqueeze(2).broadcast_to([128, 16, 16])
        cimb = cim.unsqueeze(2).broadcast_to([128, 16, 16])
        tt(bbr, braw, creb, ALU.mult, [bB, bw], [bB])
        tt(p1, biraw, cimb, ALU.mult, [bB, bw], [bB])
        tt(bbr, bbr, p1, ALU.subtract, [bB], [bB])
        tt(bbi, biraw, creb, ALU.mult, [bB, bw], [bB])
        tt(p1, braw, cimb, ALU.mult, [bB, bw], [bB])
        tt(bbi, bbi, p1, ALU.add, [bB], [bB])
        for (src, dstl) in ((bbr, Bre_l), (bbi, Bim_l)):
            memset(Wz, 0.0, [bWz])
            Wz4 = Wz.rearrange("p (g j) c -> p g j c", g=4)
            src4 = src.rearrange("p (g j) h -> p g j h", g=4)
            for jj in range(4):
                copy(Wz4[0:64, :, jj, 32 * jj:32 * jj + 16], src4[0:64, :, jj, :], [bB], [bWz])
                copy(Wz4[64:128, :, jj, 32 * jj + 16:32 * jj + 32], src4[64:128, :, jj, :], [bB], [bWz])
            for G in range(4):
                bi = G % 2
                for jj in range(4):
                    tr(PB[bi][:, jj * 128:(jj + 1) * 128], Wz[:, G * 4 + jj, :], ident[:], [bWz, bconst], [bPB[bi]],
                       sig=(jj == 3))
                copy(dstl[:, G * 4:(G + 1) * 4, :].rearrange("p a b -> p (a b)"), PB[bi], [bPB[bi]], [blhs], eng='act')
        for (srcd, dstl, sgn) in ((c_re, Cre_l, 1.0), (c_im, CimN_l, -1.0)):
            memset(Vz, 0.0, [bVz])
            for g2 in range(2):
                P.dma('sp', Vz[16 * g2:16 * g2 + 16, :, 64 * g2:64 * g2 + 64],
                      srcd[l].rearrange("(j g) h p -> g h j p", g=2)[g2], writes=[bVz])
            bi = 2
            for j in range(16):
                tr(PB[bi][:, j * 32:(j + 1) * 32], Vz[:, j, :], ident[0:32, 0:32], [bVz, bconst], [bPB[bi]], sig=(j == 15))
            act(dstl.rearrange("p a b -> p (a b)"), PB[bi], AF.Copy, [bPB[bi]], [blhs], scale=sgn)
            if dstl is Cre_l:
                act(CreN_l.rearrange("p a b -> p (a b)"), PB[bi], AF.Copy, [bPB[bi]], [blhs], scale=-1.0)
        A.off = scr0
        bscr = [bB, bWz, bVz, bang]
        P.barrier(['dve', 'act', 'pe', 'sp', 'pool'])
        hl_re = A.f32([16])
        hl_im = A.f32([16])
        bhl = [Buf() for _ in range(4)]
        memset(hl_re, 0.0, bhl)
        memset(hl_im, 0.0, bhl)
        NW = 4
        wk = []
        for i in range(NW):
            blk = A.f32([6, 4, 128])
            d_ = dict(ta=blk[:, 0], tb=blk[:, 1], zre=blk[:, 2], zim=blk[:, 3], qre=blk[:, 4], qim=blk[:, 5],
                      pb4=A.bf16([4, 4, 128]), blk=blk,
                      bta=Buf(), btb=Buf(), bzr=Buf(), bzi=Buf(), bq=Buf(), bhb=Buf())
            wk.append(d_)
        W0 = wk[0]
        yv = W0['blk'][:, 0].rearrange("p a b -> p (a b)")
        y2 = W0['blk'][:, 1].rearrange("p a b -> p (a b)")
        ysg = W0['blk'][:, 2].rearrange("p a b -> p (a b)")
        gsg = W0['blk'][:, 3].rearrange("p a b -> p (a b)")
        tw = W0['blk'][:, 4:6].rearrange("p a b c -> p (a b c)")
        bgsg = W0['bzi']
        btw = W0['bq']
        W1 = wk[1]
        yg = W1['blk'][:, 0:2].rearrange("p a b c -> p (a b c)").bitcast(BF16).rearrange("p (a b) -> p a b", a=4)
        byg = W1['bta']
        W1['btb'] = byg
        ys5 = W1['blk'][:, 2:4].rearrange("p a b c -> p (a b c)").bitcast(BF16).rearrange("p (a b) -> p a b", a=4)
        bys5 = W1['bzr']
        W1['bzi'] = bys5
        def unit(tt_, c, G, W, bk):
            cs = slice(tt_ * 512 + c * 128, tt_ * 512 + (c + 1) * 128)
            pre, pim = PB[bk], PB[bk + 1]
            bpre, bpim = bPB[bk], bPB[bk + 1]
            for j in range(4):
                mm(pre[:, j * 128:(j + 1) * 128], Bre_l[:, G * 4 + j, :], uT[:, G, cs], True, True,
                   [blhs, buT[tt_]], [bpre], sig=(j == 3))
            for j in range(4):
                mm(pim[:, j * 128:(j + 1) * 128], Bim_l[:, G * 4 + j, :], uT[:, G, cs], True, True,
                   [blhs, buT[tt_]], [bpim], sig=(j == 3))
            yield
            cG = ctab[:, G * 4:(G + 1) * 4, :]
            sG = stab[:, G * 4:(G + 1) * 4, :]
            pre3 = pre.rearrange("p (a b) -> p a b", a=4)
            pim3 = pim.rearrange("p (a b) -> p a b", a=4)
            bta, btb, bzr, bzi, bq = W['bta'], W['btb'], W['bzr'], W['bzi'], W['bq']
            tt(W['ta'], pre3, cG, ALU.mult, [bpre, btab], [bta])
            yield
            tt(W['tb'], pim3, sG, ALU.mult, [bpim, btab], [btb])
            yield
            tt(W['zre'], W['ta'], W['tb'], ALU.add, [bta, btb], [bzr])
            yield
            tt(W['ta'], pim3, cG, ALU.mult, [bpim, btab], [bta])
            yield
            tt(W['tb'], pre3, sG, ALU.mult, [bpre, btab], [btb])
            yield
            tt(W['zim'], W['ta'], W['tb'], ALU.subtract, [bta, btb], [bzi])
            yield
            for (zz, qq, hl, bz) in ((W['zre'], W['qre'], hl_re, bzr), (W['zim'], W['qim'], hl_im, bzi)):
                for j in range(4):
                    jj = G * 4 + j
                    P.op('dve', lambda e, zz=zz, qq=qq, hl=hl, j=j, jj=jj: e.tensor_tensor_scan(
                        out=qq[:, j, :], data0=rr[:, jj:jj + 1].broadcast_to([128, 128]), data1=zz[:, j, :],
                        initial=hl[:, jj:jj + 1], op0=ALU.mult, op1=ALU.add),
                        reads=[bz, bw, bhl[G]], writes=[bq], sig=(j == 3))
                yield
            pb4 = W['pb4']
            prods = ((W['ta'], W['qre'], cG, bta), (W['tb'], W['qim'], sG, btb),
                     (W['zre'], W['qre'], sG, bzr), (W['zim'], W['qim'], cG, bzi))
            for pi, (dst, qq, tab, bd) in enumerate(prods):
                tt(dst, qq, tab, ALU.mult, [bq, btab], [bd])
                copy(pb4[:, pi], dst, [bd], [W['bhb']], eng='act')
                yield
            tt(hl_re[:, G * 4:(G + 1) * 4], W['ta'][:, :, 127], W['tb'][:, :, 127], ALU.subtract, [bta, btb], [bhl[G]])
            yield
            tt(hl_im[:, G * 4:(G + 1) * 4], W['zre'][:, :, 127], W['zim'][:, :, 127], ALU.add, [bzr, bzi], [bhl[G]])
            yield
            lhs = (Cre_l, CreN_l, CimN_l, CimN_l)
            for j in range(4):
                for pi in range(4):
                    mm(PB[G][32 * j:32 * j + 32, c * 128:(c + 1) * 128], lhs[pi][:, G * 4 + j, :], pb4[:, pi, j, :],
                       pi == 0, pi == 3, [blhs, W['bhb']], [bPB[G]], sig=(j == 3 and pi == 3), tile_position=(0, 32 * j))

        wi = 0
        for tt_ in range(4):
            hs = slice(tt_ * 512, (tt_ + 1) * 512)
            for c in range(4):
                for Gp in (0, 2):
                    gens = [unit(tt_, c, Gp, wk[wi % NW], 4), unit(tt_, c, Gp + 1, wk[(wi + 1) % NW], 6)]
                    wi += 2
                    while gens:
                        for g_ in list(gens):
                            try:
                                next(g_)
                            except StopIteration:
                                gens.remove(g_)
            for G in range(4):
                stt(yv, uT[:, G, hs], small[:, 8 + G:9 + G], PB[G], ALU.mult, ALU.add, [buT[tt_], bsmall, bPB[G]],
                    [W0['bta']])
                act(y2, yv, AF.Square, [W0['bta']], [W0['btb']])
                ts(y2, y2, 0.044715, ALU.mult, [W0['btb']], [W0['btb']], s2=1.0, op1=ALU.add)
                tt(y2, y2, yv, ALU.mult, [W0['btb'], W0['bta']], [W0['btb']])
                act(ysg, y2, AF.Sigmoid, [W0['btb']], [W0['bzr']], scale=1.5957691216057308)
                tt(yg[:, G, :], yv, ysg, ALU.mult, [W0['bta'], W0['bzr']], [byg])
            for m in range(4):
                bi = 4 + m % 2
                for k in range(4):
                    mm(PB[bi], wgl[:, k, m * 128:(m + 1) * 128], yg[:, k, :], k == 0, k == 3, [bwgl, byg], [bPB[bi]])
                act(gsg, PB[bi], AF.Sigmoid, [bPB[bi], bsmall], [bgsg], bias=small[:, 12 + m:13 + m])
                tt(ys5[:, m, :], yg[:, m, :], gsg, ALU.mult, [byg, bgsg], [bys5])
            if DBG and tt_ == 0 and l == 0 and s == 0:
                dbg_dump("ys5", ys5, [128, 4, 512], bys5, BF16)
            for sub in range(4):
                t = tt_ * 4 + sub
                pp = 2 + sub % 2
                for half in range(2):
                    bi = pp * 2 + half
                    for k in range(4):
                        mm(PB[bi], ys5[:, k, sub * 128:(sub + 1) * 128], wo[:, k, half * 512:(half + 1) * 512],
                           k == 0, k == 3, [bys5, bwo], [bPB[bi]])
                tt(tw, pst[pp][:, :], gt_bc[:], ALU.mult, [bPB[pp * 2], bPB[pp * 2 + 1], bgt], [btw])
                tt(x_sb[:, t, :], x_sb[:, t, :], tw, ALU.add, [bx[t], btw], [bx[t]])
                layer_norm_tile(t)

    def ffn(l, s):
        A.reset()
        moe = (l % 2 == 1)
        nexp = NE if moe else 1
        i_l = l // 2
        wg_d = moe_wg[i_l] if moe else ffn_wg[i_l]
        wu_d = moe_wu[i_l] if moe else ffn_wu[i_l]
        wd_d = moe_wd[i_l] if moe else ffn_wd[i_l]
        P.dma('sp', gt_bc[:], gt_scr[l, 1, s].partition_broadcast(128), writes=[bgt])
        load_bcast(g_bc, bgb, ln_ffn_g[l])
        load_bcast(b_bc, bbb, ln_ffn_b[l])
        hT = A.bf16([8, SEQ])
        bhT = [Buf() for _ in range(4)]
        NWB = 3
        wgt = [A.bf16([8, 256]) for _ in range(NWB)]
        wut = [A.bf16([8, 256]) for _ in range(NWB)]
        wdt = [A.bf16([2, D]) for _ in range(NWB)]
        bwgu = [Buf() for _ in range(NWB)]
        bwd = [Buf() for _ in range(NWB)]
        actt = [A.bf16([2, 512]) for _ in range(2)]
        bact = [Buf(), Buf()]
        sgt = [A.f32([512]) for _ in range(2)]
        bsgt = [Buf(), Buf()]
        comb = A.f32([16, NE])
        bcomb = Buf()
        if moe:
            h2f = A.f32([8, 512])
            bh2f = Buf()
            wr = A.f32([8, NE])
            bwr = Buf()
            P.dma('sp', wr, w_router[i_l].rearrange("(k p) e -> p k e", p=128), writes=[bwr])
            brt = A.f32([NE])
            P.dma('sp', brt, b_router[i_l].partition_broadcast(128), writes=[bwr])
            rw = A.f32([8, NE])
            brw = Buf()
        def prologue(tt_):
            make_hT(l, 4, 3, tt_, hT[:, :, tt_ * 512:(tt_ + 1) * 512], bhT[tt_],
                    f32_copy=(h2f if moe else None), bf32=(bh2f if moe else None))
            for sub in range(4):
                t = tt_ * 4 + sub
                if moe:
                    bi = 2 + sub % 2
                    for k in range(8):
                        mm(PB[bi][:, 0:NE], h2f[:, k, sub * 128:(sub + 1) * 128], wr[:, k, :], k == 0, k == 7,
                           [bh2f, bwr], [bPB[bi]])
                    lg = rw[:, 0, :]
                    tt(lg, PB[bi][:, 0:NE], brt, ALU.add, [bPB[bi], bwr], [brw])
                    m1 = rw[:, 1, 0:1]
                    P.op('dve', lambda e, lg=lg, m1=m1: e.reduce_max(out=m1, in_=lg, axis=mybir.AxisListType.X),
                         reads=[brw], writes=[brw])
                    eq = rw[:, 2, :]
                    ts(eq, lg, m1, ALU.is_equal, [brw], [brw])
                    l2 = rw[:, 3, :]
                    stt(l2, eq, -1e30, lg, ALU.mult, ALU.add, [brw], [brw])
                    m2 = rw[:, 1, 1:2]
                    P.op('dve', lambda e, l2=l2, m2=m2: e.reduce_max(out=m2, in_=l2, axis=mybir.AxisListType.X),
                         reads=[brw], writes=[brw])
                    sel = rw[:, 4, :]
                    ts(sel, lg, m2, ALU.is_ge, [brw], [brw])
                    nm1 = rw[:, 1, 2:3]
                    ts(nm1, m1, -1.0, ALU.mult, [brw], [brw])
                    ex = rw[:, 5, :]
                    act(ex, lg, AF.Exp, [brw], [brw], bias=nm1)
                    tt(ex, ex, sel, ALU.mult, [brw], [brw])
                    ssum = rw[:, 1, 3:4]
                    P.op('dve', lambda e, ex=ex, ssum=ssum: e.reduce_sum(out=ssum, in_=ex, axis=mybir.AxisListType.X),
                         reads=[brw], writes=[brw])
                    P.op('dve', lambda e, ssum=ssum: e.reciprocal(out=ssum, in_=ssum), reads=[brw], writes=[brw])
                    ts(comb[:, t, :], ex, ssum, ALU.mult, [brw], [bcomb])
                ts(x_sb[:, t, :], x_sb[:, t, :], ALPHA, ALU.mult, [bx[t]], [bx[t]])
        NFG = DFF // 256
        pieces = [(e, fg) for e in range(nexp) for fg in range(NFG)]

        def load_piece(i):
            e, fg = pieces[i]
            bi = i % NWB
            P.dma('pool', wgt[bi], wg_d[e, :, fg * 256:(fg + 1) * 256].rearrange("(k p) n -> p k n", p=128),
                  writes=[bwgu[bi]])
            P.dma('pool', wut[bi], wu_d[e, :, fg * 256:(fg + 1) * 256].rearrange("(k p) n -> p k n", p=128),
                  writes=[bwgu[bi]])
            P.dma('pool', wdt[bi], wd_d[e, fg * 256:(fg + 1) * 256, :].rearrange("(k p) n -> p k n", p=128),
                  writes=[bwd[bi]])
            P.op('pool', lambda en: en.tensor_tensor(out=wdt[bi], in0=wdt[bi],
                                                     in1=gt_bc[:].unsqueeze(1).broadcast_to([128, 2, D]), op=ALU.mult),
                 reads=[bwd[bi], bgt], writes=[bwd[bi]])

        pend = []

        def down(i, tt_, ai):
            e, fg = pieces[i]
            bi = i % NWB
            for sub in range(4):
                t = tt_ * 4 + sub
                pp = 2 + sub % 2
                for half in range(2):
                    pbi = pp * 2 + half
                    for fc in range(2):
                        mm(PB[pbi], actt[ai][:, fc, sub * 128:(sub + 1) * 128], wdt[bi][:, fc, half * 512:(half + 1) * 512],
                           fc == 0, fc == 1, [bact[ai], bwd[bi]], [bPB[pbi]])
                sc = comb[:, t, e:e + 1] if moe else 1.0
                stt(x_sb[:, t, :], pst[pp][:, :], sc, x_sb[:, t, :], ALU.mult, ALU.add,
                    [bPB[pp * 2], bPB[pp * 2 + 1], bcomb, bx[t]], [bx[t]])
                if i == len(pieces) - 1:
                    layer_norm_tile(t)
                    if l == N_LAYER - 1:
                        P.dma('sp', out_d[s, t * 128:(t + 1) * 128, :], x_sb[:, t, :], reads=[bx[t]], writes=[Buf()])

        load_piece(0)
        if len(pieces) > 1:
            load_piece(1)
        prologue(0)
        ai = 0
        for i in range(len(pieces)):
            bi = i % NWB
            for tt_ in range(4):
                hs = slice(tt_ * 512, (tt_ + 1) * 512)
                a = ai % 2
                ai += 1
                for fc in range(2):
                    gb, ubk = fc * 2, fc * 2 + 1
                    for k in range(8):
                        mm(PB[gb], wgt[bi][:, k, fc * 128:(fc + 1) * 128], hT[:, k, hs], k == 0, k == 7,
                           [bwgu[bi], bhT[tt_]], [bPB[gb]])
                    for k in range(8):
                        mm(PB[ubk], wut[bi][:, k, fc * 128:(fc + 1) * 128], hT[:, k, hs], k == 0, k == 7,
                           [bwgu[bi], bhT[tt_]], [bPB[ubk]])
                    act(sgt[fc], PB[gb], AF.Silu, [bPB[gb]], [bsgt[fc]])
                    tt(actt[a][:, fc, :], PB[ubk], sgt[fc], ALU.mult, [bPB[ubk], bsgt[fc]], [bact[a]])
                if pend:
                    down(*pend.pop(0))
                if tt_ == 0 and i + 2 < len(pieces):
                    load_piece(i + 2)
                if i == 0 and tt_ + 1 < 4:
                    prologue(tt_ + 1)
                pend.append((i, tt_, a))
        while pend:
            down(*pend.pop(0))
        if DBG and l == 1 and s == 0 and moe:
            dbg_dump("comb", comb, [128, 16, NE], bcomb)

    for s in range(N_SEQ):
        cur_s[0] = s
        if s > 0:
            load_x(s)
        for l in range(N_LAYER):
            uT, buT = pass_a(l, s)
            if DBG and STOP_AFTER == "A":
                break
            P.barrier()
            pass_b(l, s, uT, buT)
            if DBG and STOP_AFTER == "B":
                break
            P.barrier()
            ffn(l, s)
            P.barrier()
        if DBG and STOP_AFTER:
            for t in range(16):
                P.dma('sp', out_d[s, t * 128:(t + 1) * 128, :], x_sb[:, t, :], reads=[bx[t]], writes=[Buf()])
    P.finish()
    return nc, P, list(dbg_outs)


_CACHE = {}


def _consts():
    ident = np.eye(128, dtype=np.float32)
    j = np.arange(128)
    mask = (j[:, None] <= j[None, :]).astype(np.float32)
    m0 = np.ones((128, 512), np.float32)
    m0[:, ::128] = 0.0
    tau = np.tile(np.arange(1, 129, dtype=np.float32)[None, :], (128, 1))
    return dict(k_ident=ident, k_mask=mask, k_m0=m0, k_tau=tau)


def kernel(**inputs):
    if "nc" not in _CACHE:
        _CACHE["nc"] = build_program()
    nc, P, dbg_names = _CACHE["nc"]
    f = lambda k: np.ascontiguousarray(np.asarray(inputs[k], dtype=np.float32))
    x = f("x")
    c = f("c")
    shared = {}
    for k in ["mod_w", "mod_b", "w_in", "w_out", "s5_log_dt", "s5_c_re", "s5_c_im", "s5_d", "s5_w_glu", "s5_b_glu",
              "gla_w_alpha_up", "gla_b_alpha", "gla_head_gain", "ln_mix_g", "ln_mix_b", "moe_w_router", "moe_b_router",
              "moe_w_gate", "moe_w_up", "moe_w_down", "ln_ffn_g", "ln_ffn_b"]:
        shared[k] = f(k)
    for k in ["s5_lam_re", "s5_lam_im"]:
        shared[k] = f(k).reshape(DEPTH, 2048)
    for k in ["s5_b_re", "s5_b_im"]:
        shared[k] = f(k).reshape(DEPTH, 32 * 64 * 16)
    for k in ["ffn_w_gate", "ffn_w_up", "ffn_w_down"]:
        a = f(k)
        shared[k] = a.reshape((1, 1) + a.shape[1:])
    shared.update(_consts())
    in_maps = []
    for i in range(NCORES):
        m = dict(shared)
        m["x"] = np.ascontiguousarray(x[i * SPC:(i + 1) * SPC])
        ci = c[i * SPC:(i + 1) * SPC]
        m["cT"] = np.ascontiguousarray(ci.T.reshape(8, 128, SPC).transpose(1, 0, 2))
        in_maps.append(m)
    res = run_bass_kernel_spmd(nc, in_maps, core_ids=list(range(NCORES)))
    out = np.concatenate([np.asarray(r["out"]) for r in res.results], axis=0).astype(np.float32)
    if DBG:
        _CACHE["dbg"] = [{n: np.asarray(r["dbg_" + n]) for n in dbg_names} for r in res.results]
    return out
```

```python
import math
import os
from contextlib import ExitStack

import numpy as np
import concourse.bass as bass
import concourse.mybir as mybir
from concourse.bass_utils import run_bass_kernel_spmd

F32 = mybir.dt.float32
BF16 = mybir.dt.bfloat16
I32 = mybir.dt.int32
AF = mybir.ActivationFunctionType
ALU = mybir.AluOpType

D = 1024
SEQ = 2048
NB = 32
DEPTH = 2
NCORES = 8
SPC = NB // NCORES
INC = 2064
DFF = 3584
NE = 8
NMOD = 6
ALPHA = (2.0 * DEPTH) ** 0.25
LN_EPS = 1e-5
RMS_EPS = 1e-6
TWO_PI = 2.0 * math.pi

ENGS = ['pe', 'act', 'dve', 'pool', 'sp']
SEM_EPOCH = 12000
DMA_SEMS = dict(sp=10, pool=14, act=4, dve=2, pe=2)
SAME_ENGINE_SYNC = True
ROT_ENG = os.environ.get("MK_ROT_ENG", "dve")
LN_ENG = os.environ.get("MK_LN_ENG", "pool")

DBG = os.environ.get("MK_DEBUG", "")
N_SEQ = int(os.environ.get("MK_NSEQ", str(SPC)))
N_LAYER = int(os.environ.get("MK_NLAYER", str(DEPTH)))
STOP_AFTER = os.environ.get("MK_STOP", "")


class Buf:
    __slots__ = ('name', 'writers', 'readers')

    def __init__(self, name=''):
        self.name = name
        self.writers = {}
        self.readers = {}


def _merge(dst, src):
    for k, v in src.items():
        if dst.get(k, -1) < v:
            dst[k] = v


class Prog:
    def __init__(self, nc):
        self.nc = nc
        self.stack = ExitStack()
        self.sems = []
        self.eng = dict(pe=nc.tensor, act=nc.scalar, dve=nc.vector, pool=nc.gpsimd, sp=nc.sync)
        self.esem = {e: None for e in ENGS}
        self.known = {e: {} for e in ENGS}
        self.pending = {e: ([], []) for e in ENGS}
        self.dsem = {}
        self.ninst = {e: 0 for e in ENGS}
        self.all_dma = {}

    def new_sem(self):
        i = len(self.sems)
        self.sems.append(self.stack.enter_context(self.nc.semaphore(f"s{i}")))
        return i

    def tile(self, name, shape, dt):
        return self.stack.enter_context(self.nc.sbuf_tensor(name, list(shape), dt))

    def psum(self, name, shape, dt=F32):
        return self.stack.enter_context(self.nc.psum_tensor(name, list(shape), dt))

    def _wait(self, e, deps):
        eng = self.eng[e]
        kn = self.known[e]
        for (semid, val) in deps.items():
            if kn.get(semid, 0) >= val:
                continue
            eng.wait_ge(self.sems[semid], val)
            kn[semid] = val
            self.ninst[e] += 1

    def _collect(self, reads, writes):
        deps = {}
        for b in reads:
            _merge(deps, b.writers)
        for b in writes:
            _merge(deps, b.writers)
            _merge(deps, b.readers)
        return deps

    def _next_sig(self, e):
        cur = self.esem[e]
        if cur is None or cur[1] >= SEM_EPOCH:
            cur = (self.new_sem(), 0)
        cur = (cur[0], cur[1] + 1)
        self.esem[e] = cur
        return cur

    def op(self, e, fn, reads=(), writes=(), sig=True):
        deps = self._collect(reads, writes)
        if e == 'pe' or not SAME_ENGINE_SYNC:
            cur = self.esem[e]
            if cur is not None:
                deps.pop(cur[0], None)
        self._wait(e, deps)
        ins = fn(self.eng[e])
        self.ninst[e] += 1
        pr, pw = self.pending[e]
        if not sig:
            pr.extend(reads)
            pw.extend(writes)
            return ins
        semid, val = self._next_sig(e)
        ins.then_inc(self.sems[semid], 1)
        me = {semid: val}
        for b in list(pr) + list(reads):
            _merge(b.readers, me)
        for b in list(pw) + list(writes):
            b.writers = dict(me)
            b.readers = {}
        pr.clear()
        pw.clear()
        return ins

    def dma(self, q, out, in_, reads=(), writes=(), **kw):
        deps = self._collect(reads, writes)
        st = self.dsem.get(q)
        if st is None:
            k = DMA_SEMS[q]
            st = dict(sems=[self.new_sem() for _ in range(k)], vals=[0] * k, rr=0)
            self.dsem[q] = st
        j = st['rr'] % len(st['sems'])
        st['rr'] += 1
        semid = st['sems'][j]
        prev = st['vals'][j]
        if prev > 0 and deps.get(semid, 0) < prev:
            deps[semid] = prev
        self._wait(q, deps)
        val = prev + 16
        st['vals'][j] = val
        kw.setdefault('allow_slow_non_contiguous', True)
        self.eng[q].dma_start(out=out, in_=in_, **kw).then_inc(self.sems[semid], 16)
        self.ninst[q] += 1
        me = {semid: val}
        _merge(self.all_dma, me)
        for b in reads:
            _merge(b.readers, me)
        for b in writes:
            b.writers = dict(me)
            b.readers = {}

    def barrier(self, engines=ENGS):
        deps = dict(self.all_dma)
        for e in ENGS:
            cur = self.esem[e]
            if cur is not None:
                deps[cur[0]] = cur[1]
        for e in engines:
            self._wait(e, dict(deps))

    def finish(self):
        self.barrier(['sp'])
        self.stack.close()


class Arena:
    def __init__(self, tile, n):
        self.t = tile
        self.n = n
        self.off = 0

    def reset(self):
        self.off = 0

    def f32(self, shape, parts=128):
        n = int(np.prod(shape))
        assert self.off + n <= self.n, ("arena overflow", self.off, n, self.n)
        v = self.t[0:parts, self.off:self.off + n]
        self.off += n
        return self._shape(v, shape)

    def bf16(self, shape, parts=128):
        n = int(np.prod(shape))
        n32 = (n + 1) // 2
        assert self.off + n32 <= self.n, ("arena overflow", self.off, n32, self.n)
        v = self.t[0:parts, self.off:self.off + n32].bitcast(BF16)
        if n32 * 2 != n:
            v = v[:, 0:n]
        self.off += n32
        return self._shape(v, shape)

    def i32(self, shape, parts=128):
        n = int(np.prod(shape))
        v = self.t[0:parts, self.off:self.off + n].bitcast(I32)
        self.off += n
        return self._shape(v, shape)

    @staticmethod
    def _shape(v, shape):
        if len(shape) == 1:
            return v
        if len(shape) == 2:
            return v.rearrange("p (a b) -> p a b", a=shape[0])
        if len(shape) == 3:
            return v.rearrange("p (a b c) -> p a b c", a=shape[0], b=shape[1])
        if len(shape) == 4:
            return v.rearrange("p (a b c d) -> p a b c d", a=shape[0], b=shape[1], c=shape[2])
        raise ValueError(shape)


def build_program():
    nc = bass.Bass("TRN2", target_bir_lowering=False)
    P = Prog(nc)

    def din(name, shape, dt=F32):
        return nc.dram_tensor(name, list(shape), dt, kind="ExternalInput").ap()

    x_d = din("x", [SPC, SEQ, D])
    cT_d = din("cT", [128, 8, SPC])
    mod_w = din("mod_w", [DEPTH, D, NMOD * D])
    mod_b = din("mod_b", [DEPTH, NMOD * D])
    w_in = din("w_in", [DEPTH, D, INC])
    w_out = din("w_out", [DEPTH, D, D])
    lam_re = din("s5_lam_re", [DEPTH, 2048])
    lam_im = din("s5_lam_im", [DEPTH, 2048])
    log_dt = din("s5_log_dt", [DEPTH, 32])
    b_re = din("s5_b_re", [DEPTH, 32 * 64 * 16])
    b_im = din("s5_b_im", [DEPTH, 32 * 64 * 16])
    c_re = din("s5_c_re", [DEPTH, 32, 16, 64])
    c_im = din("s5_c_im", [DEPTH, 32, 16, 64])
    s5_d = din("s5_d", [DEPTH, 512])
    w_glu = din("s5_w_glu", [DEPTH, 512, 512])
    b_glu = din("s5_b_glu", [DEPTH, 512])
    w_aup = din("gla_w_alpha_up", [DEPTH, 16, 256])
    b_alpha = din("gla_b_alpha", [DEPTH, 256])
    head_gain = din("gla_head_gain", [DEPTH, 512])
    ln_mix_g = din("ln_mix_g", [DEPTH, D])
    ln_mix_b = din("ln_mix_b", [DEPTH, D])
    ffn_wg = din("ffn_w_gate", [1, 1, D, DFF])
    ffn_wu = din("ffn_w_up", [1, 1, D, DFF])
    ffn_wd = din("ffn_w_down", [1, 1, DFF, D])
    w_router = din("moe_w_router", [1, D, NE])
    b_router = din("moe_b_router", [1, NE])
    moe_wg = din("moe_w_gate", [1, NE, D, DFF])
    moe_wu = din("moe_w_up", [1, NE, D, DFF])
    moe_wd = din("moe_w_down", [1, NE, DFF, D])
    ln_ffn_g = din("ln_ffn_g", [DEPTH, D])
    ln_ffn_b = din("ln_ffn_b", [DEPTH, D])
    ident_d = din("k_ident", [128, 128])
    mask_d = din("k_mask", [128, 128])
    m0_d = din("k_m0", [128, 512])
    tau_d = din("k_tau", [128, 128])
    out_d = nc.dram_tensor("out", [SPC, SEQ, D], F32, kind="ExternalOutput").ap()
    gt_scr = nc.dram_tensor("gt_scr", [DEPTH, 2, SPC, D], F32, kind="Internal").ap()
    dbg_outs = {}

    def dbg_dump(name, ap_sb, shape, buf, dt=F32):
        if not DBG:
            return
        d = nc.dram_tensor("dbg_" + name, list(shape), dt, kind="ExternalOutput").ap()
        dbg_outs[name] = d
        P.dma('sp', d, ap_sb, reads=[buf], writes=[Buf()])

    x_sb = P.tile("x_sb", [128, 16, D], F32)
    bx = [Buf(f"x{t}") for t in range(16)]
    ARENA_N = 30848
    arena_t = P.tile("arena", [128, ARENA_N], F32)
    A = Arena(arena_t, ARENA_N)
    ident = P.tile("ident", [128, 128], F32)
    identb = P.tile("identb", [128, 128], BF16)
    mask128 = P.tile("mask128", [128, 128], F32)
    m0tab = P.tile("m0tab", [128, 512], F32)
    tau1 = P.tile("tau1", [128, 128], F32)
    onesdiv = P.tile("onesdiv", [128, 128], F32)
    ones_row = P.tile("ones_row", [1, 128], F32)
    eps_ln = P.tile("eps_ln", [128, 1], F32)
    eps_rms = P.tile("eps_rms", [128, 1], F32)
    cact = P.tile("cact", [128, 8, SPC], F32)
    modT = P.tile("modT", [128, DEPTH, 48, SPC], F32)
    gt_bc = P.tile("gt_bc", [128, D], F32)
    g_bc = P.tile("g_bc", [128, D], F32)
    b_bc = P.tile("b_bc", [128, D], F32)
    small = P.tile("small", [128, 256], F32)
    bconst = Buf("const")
    bmod = Buf("mod")
    bgt, bgb, bbb = Buf("gt_bc"), Buf("g_bc"), Buf("b_bc")
    bsmall = Buf("small")

    pst = [P.psum(f"ps{i}", [128, 1024]) for i in range(4)]
    PB = [pst[i // 2][:, (i % 2) * 512:(i % 2) * 512 + 512] for i in range(8)]
    bPB = [Buf(f"bank{i}") for i in range(8)]

    def mm(out, lhsT, rhs, start, stop, reads, writes, sig=None, **kw):
        if sig is None:
            sig = stop
        return P.op('pe', lambda e: e.matmul(out, lhsT=lhsT, rhs=rhs, start=start, stop=stop, **kw),
                    reads=reads, writes=writes, sig=sig)

    def tr(out, in_, idt, reads, writes, sig=True):
        return P.op('pe', lambda e: e.transpose(out=out, in_=in_, identity=idt), reads=reads, writes=writes, sig=sig)

    def act(out, in_, func, reads, writes, scale=None, bias=None):
        kw = {}
        if scale is not None:
            kw['scale'] = scale
        if bias is not None:
            kw['bias'] = bias
        return P.op('act', lambda e: e.activation(out=out, in_=in_, func=func, **kw), reads=reads, writes=writes)

    def tt(out, in0, in1, op, reads, writes, eng='dve'):
        return P.op(eng, lambda e: e.tensor_tensor(out=out, in0=in0, in1=in1, op=op), reads=reads, writes=writes)

    def ts(out, in0, s1, op0, reads, writes, s2=None, op1=None, eng='dve'):
        if op1 is None:
            return P.op(eng, lambda e: e.tensor_scalar(out=out, in0=in0, scalar1=s1, scalar2=None, op0=op0),
                        reads=reads, writes=writes)
        return P.op(eng, lambda e: e.tensor_scalar(out=out, in0=in0, scalar1=s1, scalar2=s2, op0=op0, op1=op1),
                    reads=reads, writes=writes)

    def stt(out, in0, scalar, in1, op0, op1, reads, writes):
        return P.op('dve', lambda e: e.scalar_tensor_tensor(out=out, in0=in0, scalar=scalar, in1=in1, op0=op0, op1=op1),
                    reads=reads, writes=writes)

    def copy(out, in_, reads, writes, eng='dve'):
        if eng == 'act':
            return act(out, in_, AF.Copy, reads, writes)
        return P.op(eng, lambda e: e.tensor_copy(out=out, in_=in_), reads=reads, writes=writes)

    def memset(out, val, writes, eng='dve'):
        return P.op(eng, lambda e: e.memset(out, val), reads=(), writes=writes)

    def load_x(s_):
        for t in range(16):
            P.dma('sp', x_sb[:, t, :], x_d[s_, t * 128:(t + 1) * 128, :], writes=[bx[t]])

    load_x(0)
    P.dma('sp', ident[:], ident_d, writes=[bconst])
    P.dma('sp', mask128[:], mask_d, writes=[bconst])
    P.dma('sp', m0tab[:], m0_d, writes=[bconst])
    P.dma('sp', tau1[:], tau_d, writes=[bconst])
    copy(identb[:], ident[:], [bconst], [bconst])
    memset(onesdiv[:], 1.0 / 128.0, [bconst])
    memset(ones_row[:], 1.0, [bconst])
    memset(eps_ln[:], LN_EPS, [bconst])
    memset(eps_rms[:], RMS_EPS, [bconst])
    A.reset()
    ctmp = A.f32([8, SPC])
    bct = Buf()
    P.dma('sp', ctmp, cT_d, writes=[bct])
    act(cact[:], ctmp, AF.Silu, [bct], [bconst])

    mb = A.f32([NMOD * D], parts=1)
    mw = [A.f32([8, 512]) for _ in range(2)]
    gtr = [A.f32([512], parts=SPC) for _ in range(2)]
    bmb = Buf()
    bmw = [Buf(), Buf()]
    bgtr = [Buf(), Buf()]
    it = 0
    for l in range(DEPTH):
        P.dma('sp', mb, mod_b[l:l + 1, :], writes=[bmb])
        for cb in range(12):
            bi = it % 2
            it += 1
            which = cb // 2
            P.dma('sp', mw[bi], mod_w[l, :, cb * 512:(cb + 1) * 512].rearrange("(k p) n -> p k n", p=128),
                  writes=[bmw[bi]])
            pb = PB[it % 2]
            bpb = bPB[it % 2]
            for cc in range(4):
                for k in range(8):
                    mm(pb[:, cc * 4:cc * 4 + 4], mw[bi][:, k, cc * 128:(cc + 1) * 128], cact[:, k, :],
                       k == 0, False, [bmw[bi], bconst], [bpb], sig=False)
                mm(pb[:, cc * 4:cc * 4 + 4], mb[0:1, cb * 512 + cc * 128:cb * 512 + (cc + 1) * 128],
                   ones_row[0:1, 0:SPC], False, True, [bmb, bconst], [bpb], sig=True)
            dst = modT[:, l, cb * 4:(cb + 1) * 4, :]
            src = pb[:, 0:16].rearrange("p (a b) -> p a b", a=4)
            if which in (1, 2, 4, 5):
                ts(dst, src, 1.0, ALU.add, [bpb], [bmod])
            else:
                copy(dst, src, [bpb], [bmod])
            if which in (2, 5):
                pr_ = PB[2 + it % 2]
                bpr = bPB[2 + it % 2]
                for k in range(8):
                    mm(pr_[0:SPC, :], cact[:, k, :], mw[bi][:, k, :], k == 0, False, [bmw[bi], bconst], [bpr], sig=False)
                mm(pr_[0:SPC, :], ones_row[0:1, 0:SPC], mb[0:1, cb * 512:(cb + 1) * 512], False, True,
                   [bmb, bconst], [bpr], sig=True)
                ts(gtr[bi], pr_[0:SPC, :], 1.0, ALU.add, [bpr], [bgtr[bi]])
                wsel = 0 if which == 2 else 1
                half = cb % 2
                P.dma('sp', gt_scr[l, wsel, :, half * 512:(half + 1) * 512], gtr[bi], reads=[bgtr[bi]], writes=[bgt])
    P.barrier()

    def load_bcast(dst, bdst, src_vec):
        P.dma('sp', dst[:], src_vec.partition_broadcast(128), writes=[bdst])

    def make_hT(l, which_sc, which_sh, tt_, dst, bdst, f32_copy=None, bf32=None):
        for kc in range(8):
            pb = PB[kc % 2]
            bpb = bPB[kc % 2]
            for sub in range(4):
                t = tt_ * 4 + sub
                tr(pb[:, sub * 128:(sub + 1) * 128], x_sb[:, t, kc * 128:(kc + 1) * 128], ident[:],
                   [bx[t], bconst], [bpb], sig=(sub == 3))
            sc = modT[:, l, which_sc * 8 + kc, cur_s[0]:cur_s[0] + 1]
            sh = modT[:, l, which_sh * 8 + kc, cur_s[0]:cur_s[0] + 1]
            act(dst[:, kc, :], pb, AF.Identity, [bpb, bmod], [bdst], scale=sc, bias=sh)
            if f32_copy is not None:
                act(f32_copy[:, kc, :], pb, AF.Identity, [bpb, bmod], [bf32], scale=sc, bias=sh)

    cur_s = [0]

    def layer_norm_tile(t, gb_eng='dve'):
        st = lnw_stats[t % 2]
        bst = blnw[t % 2]
        P.op('dve', lambda e: e.bn_stats(out=st[:, 0:6], in_=x_sb[:, t, 0:512]), reads=[bx[t]], writes=[bst])
        P.op('dve', lambda e: e.bn_stats(out=st[:, 6:12], in_=x_sb[:, t, 512:1024]), reads=[bx[t]], writes=[bst])
        P.op('dve', lambda e: e.bn_aggr(out=st[:, 12:14], in_=st[:, 0:12]), reads=[bst], writes=[bst])
        act(st[:, 14:15], st[:, 13:14], AF.Sqrt, [bst], [bst], bias=eps_ln[:])
        P.op('dve', lambda e: e.reciprocal(out=st[:, 15:16], in_=st[:, 14:15]), reads=[bst], writes=[bst])
        stt(st[:, 16:17], st[:, 12:13], -1.0, st[:, 15:16], ALU.mult, ALU.mult, [bst], [bst])
        act(x_sb[:, t, :], x_sb[:, t, :], AF.Identity, [bst, bx[t]], [bx[t]], scale=st[:, 15:16], bias=st[:, 16:17])
        tt(x_sb[:, t, :], x_sb[:, t, :], g_bc[:], ALU.mult, [bx[t], bgb], [bx[t]], eng=gb_eng)
        tt(x_sb[:, t, :], x_sb[:, t, :], b_bc[:], ALU.add, [bx[t], bbb], [bx[t]], eng=gb_eng)

    lnw_t = P.tile("lnw", [128, 2, 32], F32)
    lnw_stats = [lnw_t[:, 0, :], lnw_t[:, 1, :]]
    blnw = [Buf(), Buf()]

    def pass_a(l, s):
        A.reset()
        uT = A.bf16([4, SEQ])
        buT = [Buf() for _ in range(4)]
        win = A.bf16([8, INC])
        w_in_v = w_in[l].rearrange("(k p) n -> p k n", p=128)
        bwin_g = {}
        for (c0, c1) in ((0, 512), (1024, 1536), (2048, INC), (512, 1024), (1536, 2048)):
            bg_ = Buf()
            P.dma('pool', win[:, :, c0:c1], w_in_v[:, :, c0:c1], writes=[bg_])
            bwin_g[c0] = bg_

        def bwin_of(col):
            if col < 512:
                return bwin_g[0]
            if col < 1024:
                return bwin_g[512]
            if col < 1536:
                return bwin_g[1024]
            if col < 2048:
                return bwin_g[1536]
            return bwin_g[2048]
        wo = A.bf16([4, D])
        bwo = Buf()
        P.dma('pool', wo, w_out[l, 512:1024, :].rearrange("(k p) n -> p k n", p=128), writes=[bwo])
        hTt = A.bf16([8, 512])
        bhTt = Buf()
        waup = A.bf16([256], parts=16)
        bwa = Buf()
        P.dma('pool', waup, w_aup[l], writes=[bwa])
        P.dma('sp', small[:, 0:2], b_alpha[l].rearrange("(h q) -> q h", q=128), writes=[bsmall],
              allow_slow_non_contiguous=True)
        P.dma('sp', small[:, 2:6], head_gain[l].rearrange("(h q) -> q h", q=128), writes=[bsmall],
              allow_slow_non_contiguous=True)
        ts(small[:, 0:2], small[:, 0:2], -1.0, ALU.mult, [bsmall], [bsmall])
        P.dma('sp', gt_bc[:], gt_scr[l, 0, s].partition_broadcast(128), writes=[bgt])
        vtok = A.bf16([4, 512])
        bvt = Buf()
        a16 = A.bf16([512], parts=16)
        ba16 = Buf()
        lsp = A.f32([2, 512])
        blsp = Buf()
        bcum = A.f32([2, 512])
        bbc = Buf()
        eb = A.f32([2, 512])
        beb = Buf()
        enb = A.f32([2, 512])
        benb = Buf()
        qT = A.bf16([2, 512])
        bqT = Buf()
        kz = [A.bf16([2, 512]) for _ in range(2)]
        bkz = [Buf(), Buf()]
        ktok = A.bf16([2, 2, 4, 128])
        bkt = Buf()
        ssb = A.bf16([2, 2, 4, 128])
        bss = [Buf(), Buf()]
        state = A.f32([4, 128])
        bstt = [Buf() for _ in range(4)]
        stbf = A.bf16([4, 128])
        bsbf = [Buf() for _ in range(4)]
        tmpT = A.f32([4, 128])
        btmp = [Buf() for _ in range(4)]
        sq = bcum[:, 0, :]
        rstd = bcum[:, 1, :]
        bsq = bbc
        brs = bbc
        t1 = enb[:, 0, :]
        sg = enb[:, 1, :]
        bt1 = benb
        bsg = benb
        ygla = A.bf16([4, 512])
        byg = Buf()
        tw = lsp.rearrange("p a b -> p (a b)")
        btw = blsp
        memset(kz[0], 0.0, [bkz[0]])
        memset(kz[1], 0.0, [bkz[1]])
        memset(state, 0.0, bstt)
        memset(stbf, 0.0, bsbf)
        for tt_ in range(4):
            make_hT(l, 1, 0, tt_, hTt, bhTt)
            hs = slice(tt_ * 512, (tt_ + 1) * 512)
            rot = [0]

            def proj(col0, m, evac):
                bi = 2 + rot[0] % 2
                rot[0] += 1
                for k in range(8):
                    mm(PB[bi][0:m, :], win[:, k, col0:col0 + m], hTt[:, k, :], k == 0, k == 7,
                       [bwin_of(col0), bhTt], [bPB[bi]])
                evac(PB[bi][0:m, :], bPB[bi])

            for c4 in range(4):
                proj(c4 * 128, 128, lambda ps, b, c4=c4: copy(uT[:, c4, hs], ps, [b], [buT[tt_]], eng='act'))
            for sub in range(4):
                bi = 2 + rot[0] % 2
                rot[0] += 1
                for k in range(8):
                    mm(PB[bi], hTt[:, k, sub * 128:(sub + 1) * 128], win[:, k, 1024:1536],
                       k == 0, k == 7, [bwin_of(1024), bhTt], [bPB[bi]])
                copy(vtok[:, sub, :], PB[bi], [bPB[bi]], [bvt], eng='act')
            proj(2048, 16, lambda ps, b: copy(a16, ps, [b], [ba16], eng='act'))
            for hp in range(2):
                bi = 2 + rot[0] % 2
                rot[0] += 1
                mm(PB[bi], waup[:, hp * 128:(hp + 1) * 128], a16, True, True, [bwa, ba16], [bPB[bi]])
                act(lsp[:, hp, :], PB[bi], AF.Exp, [bPB[bi], bsmall], [blsp], scale=-1.0, bias=small[:, hp:hp + 1])
            act(lsp, lsp, AF.Ln, [blsp], [blsp], bias=1.0)
            for hp in range(2):
                P.op('dve', lambda e, hp=hp: e.tensor_tensor_scan(out=bcum[:, hp, :], data0=m0tab[:], data1=lsp[:, hp, :],
                                                                  initial=0.0, op0=ALU.mult, op1=ALU.add),
                     reads=[blsp, bconst], writes=[bbc])
            act(eb, bcum, AF.Exp, [bbc], [beb], scale=-1.0 / 16.0)
            act(enb, bcum, AF.Exp, [bbc], [benb], scale=1.0 / 16.0)
            for hp in range(2):
                proj(512 + hp * 128, 128,
                     lambda ps, b, hp=hp: stt(qT[:, hp, :], ps, 0.125, eb[:, hp, :], ALU.mult, ALU.mult, [b, beb], [bqT]))
            for hp in range(2):
                def ev(ps, b, hp=hp):
                    for m in range(2):
                        tt(kz[m][64 * m:64 * m + 64, hp, :], ps[64 * m:64 * m + 64, :], enb[64 * m:64 * m + 64, hp, :],
                           ALU.mult, [b, benb], [bkz[m]])
                proj(768 + hp * 128, 128, ev)
            for m in range(2):
                pbb = PB[m].bitcast(BF16)
                for hp in range(2):
                    for sub in range(4):
                        idx = hp * 4 + sub
                        tr(pbb[:, idx * 128:(idx + 1) * 128], kz[m][:, hp, sub * 128:(sub + 1) * 128], identb[:],
                           [bkz[m], bconst], [bPB[m]], sig=(idx == 7))
                copy(ktok[:, m, :, :, :].rearrange("p a b c -> p (a b c)"), pbb, [bPB[m]], [bkt], eng='act')
            for hp in range(2):
                for m in range(2):
                    bi = 4 + m
                    for sub in range(4):
                        cs = slice(sub * 128, (sub + 1) * 128)
                        mm(PB[bi][:, cs], kz[m][:, hp, cs], qT[:, hp, cs], True, True, [bkz[m], bqT], [bPB[bi]],
                           sig=(sub == 3))
                    tt(ssb[:, hp, m, :, :], PB[bi].rearrange("p (a b) -> p a b", a=4),
                       mask128[:].unsqueeze(1).broadcast_to([128, 4, 128]), ALU.mult, [bPB[bi], bconst], [bss[hp]])
            for sub in range(4):
                cs = slice(sub * 128, (sub + 1) * 128)
                for h in range(4):
                    hp, m = h // 2, h % 2
                    mm(PB[h][:, cs], vtok[:, sub, h * 128:(h + 1) * 128], ssb[:, hp, m, sub, :], True, False,
                       [bvt, bss[hp]], [bPB[h]], sig=False)
                    mm(PB[h][:, cs], stbf[:, h, :], qT[:, hp, cs], False, True, [bsbf[h], bqT], [bPB[h]], sig=True)
                for h in range(4):
                    hp, m = h // 2, h % 2
                    ub = 6 + (h % 2)
                    mm(PB[ub][:, 0:128], ktok[:, m, hp, sub, :], vtok[:, sub, h * 128:(h + 1) * 128], True, True,
                       [bkt, bvt], [bPB[ub]])
                    tt(tmpT[:, h, :], PB[ub][:, 0:128], state[:, h, :], ALU.add, [bPB[ub], bstt[h]], [btmp[h]])
                    ts(state[:, h, :], tmpT[:, h, :], eb[:, hp, sub * 128 + 127: sub * 128 + 128], ALU.mult,
                       [btmp[h], beb], [bstt[h]])
                    copy(stbf[:, h, :], state[:, h, :], [bstt[h]], [bsbf[h]], eng='act')
            for h in range(4):
                act(sq, PB[h], AF.Square, [bPB[h]], [bsq])
                mb_ = 4 + h % 2
                mm(PB[mb_], onesdiv[:], sq, True, True, [bconst, bsq], [bPB[mb_]])
                act(rstd, PB[mb_], AF.Sqrt, [bPB[mb_], bconst], [brs], bias=eps_rms[:])
                P.op('dve', lambda e: e.reciprocal(out=rstd, in_=rstd), reads=[brs], writes=[brs])
                stt(t1, PB[h], small[:, 2 + h:3 + h], rstd, ALU.mult, ALU.mult, [bPB[h], bsmall, brs], [bt1])
                gb = 6 + h % 2
                for k in range(8):
                    mm(PB[gb], win[:, k, 1536 + h * 128:1536 + (h + 1) * 128], hTt[:, k, :], k == 0, k == 7,
                       [bwin_of(1536), bhTt], [bPB[gb]])
                act(sg, PB[gb], AF.Silu, [bPB[gb]], [bsg])
                tt(ygla[:, h, :], t1, sg, ALU.mult, [bt1, bsg], [byg])
            if DBG and tt_ == 0 and l == 0 and s == 0:
                dbg_dump("ygla", ygla, [128, 4, 512], byg, BF16)
                dbg_dump("uT", uT[:, :, 0:512], [128, 4, 512], buT[0], BF16)
            for sub in range(4):
                t = tt_ * 4 + sub
                pp = 2 + sub % 2
                for half in range(2):
                    bi = pp * 2 + half
                    for h in range(4):
                        mm(PB[bi], ygla[:, h, sub * 128:(sub + 1) * 128], wo[:, h, half * 512:(half + 1) * 512],
                           h == 0, h == 3, [byg, bwo], [bPB[bi]])
                tt(tw, pst[pp][:, :], gt_bc[:], ALU.mult, [bPB[pp * 2], bPB[pp * 2 + 1], bgt], [btw])
                stt(x_sb[:, t, :], x_sb[:, t, :], ALPHA, tw, ALU.mult, ALU.add, [bx[t], btw], [bx[t]])
        return uT, buT

    def pass_b(l, s, uT, buT):
        A.reset()
        A.bf16([4, SEQ])
        wo = A.bf16([4, D])
        bwo = Buf()
        P.dma('pool', wo, w_out[l, 0:512, :].rearrange("(k p) n -> p k n", p=128), writes=[bwo])
        wgl = A.bf16([4, 512])
        bwgl = Buf()
        P.dma('pool', wgl, w_glu[l].rearrange("(k p) n -> p k n", p=128), writes=[bwgl])
        load_bcast(g_bc, bgb, ln_mix_g[l])
        load_bcast(b_bc, bbb, ln_mix_b[l])
        P.dma('sp', small[:, 8:12], s5_d[l].rearrange("(c q) -> q c", q=128), writes=[bsmall],
              allow_slow_non_contiguous=True)
        P.dma('sp', small[:, 12:16], b_glu[l].rearrange("(c q) -> q c", q=128), writes=[bsmall],
              allow_slow_non_contiguous=True)
        lre = A.f32([16])
        lim = A.f32([16])
        ldt = A.f32([16])
        bl = Buf()
        P.dma('sp', lre, lam_re[l].rearrange("(j q) -> q j", q=128), writes=[bl], allow_slow_non_contiguous=True)
        P.dma('sp', lim, lam_im[l].rearrange("(j q) -> q j", q=128), writes=[bl], allow_slow_non_contiguous=True)
        for g2 in range(2):
            P.dma('sp', ldt[64 * g2:64 * g2 + 64, :],
                  log_dt[l].rearrange("(j g) -> g j", g=2)[g2].partition_broadcast(64), writes=[bl])
        dtt = A.f32([16])
        th = A.f32([16])
        rr = A.f32([16])
        w1 = A.f32([16])
        w2 = A.f32([16])
        w3 = A.f32([16])
        cre = A.f32([16])
        cim = A.f32([16])
        bw = Buf()
        act(dtt, ldt, AF.Exp, [bl], [bw])
        tt(th, lim, dtt, ALU.mult, [bl, bw], [bw])
        tt(w1, lre, dtt, ALU.mult, [bl, bw], [bw])
        act(rr, w1, AF.Exp, [bw], [bw])
        ctab = A.f32([16, 128])
        stab = A.f32([16, 128])
        nre = A.f32([16])
        nim = A.f32([16])
        Bre_l = A.bf16([16, 128])
        Bim_l = A.bf16([16, 128])
        Cre_l = A.bf16([16, 32])
        CreN_l = A.bf16([16, 32])
        CimN_l = A.bf16([16, 32])
        blhs = Buf()
        scr0 = A.off
        ang = A.f32([16, 128])
        kf = A.f32([16, 128])
        ki = A.i32([16, 128])
        btab = Buf()
        bang = Buf()
        tt(ang, th.unsqueeze(2).broadcast_to([128, 16, 128]), tau1[:].unsqueeze(1).broadcast_to([128, 16, 128]),
           ALU.mult, [bw, bconst], [bang])

        def sin_of(dst, shift):
            ts(kf, ang, 1.0 / TWO_PI, ALU.mult, [bang], [bang], s2=shift / TWO_PI, op1=ALU.add)
            copy(ki, kf, [bang], [bang])
            copy(kf, ki, [bang], [bang])
            stt(kf, kf, -TWO_PI, ang, ALU.mult, ALU.add, [bang], [bang])
            ts(kf, kf, shift, ALU.add, [bang], [bang], s2=math.pi, op1=ALU.min)
            ts(kf, kf, -math.pi, ALU.max, [bang], [bang])
            act(dst, kf, AF.Sin, [bang], [btab])

        sin_of(stab, 0.0)
        sin_of(ctab, math.pi / 2.0)
        c0 = ctab[:, :, 0]
        s0 = stab[:, :, 0]
        tt(nre, rr, c0, ALU.mult, [bw, btab], [bw])
        ts(nre, nre, -1.0, ALU.add, [bw], [bw])
        tt(nim, rr, s0, ALU.mult, [bw, btab], [bw])
        tt(w1, lre, lre, ALU.mult, [bl], [bw])
        tt(w2, lim, lim, ALU.mult, [bl], [bw])
        tt(w1, w1, w2, ALU.add, [bw], [bw])
        P.op('dve', lambda e: e.reciprocal(out=w1, in_=w1), reads=[bw], writes=[bw])
        tt(w2, nre, lre, ALU.mult, [bw, bl], [bw])
        tt(w3, nim, lim, ALU.mult, [bw, bl], [bw])
        tt(w2, w2, w3, ALU.add, [bw], [bw])
        tt(cre, w2, w1, ALU.mult, [bw], [bw])
        tt(w2, nim, lre, ALU.mult, [bw, bl], [bw])
        tt(w3, nre, lim, ALU.mult, [bw, bl], [bw])
        tt(w2, w2, w3, ALU.subtract, [bw], [bw])
        tt(cim, w2, w1, ALU.mult, [bw], [bw])
        braw = A.f32([16, 16])
        biraw = A.f32([16, 16])
        bbr = A.f32([16, 16])
        bbi = A.f32([16, 16])
        p1 = A.f32([16, 16])
        Wz = A.f32([16, 128])
        Vz = A.f32([16, 128], parts=32)
        bB = Buf()
        bWz = Buf()
        bVz = Buf()
        P.dma('sp', braw, b_re[l].rearrange("(j q h) -> q j h", q=128, h=16), writes=[bB])
        P.dma('sp', biraw, b_im[l].rearrange("(j q h) -> q j h", q=128, h=16), writes=[bB])
        creb = cre.unsqueeze(2).broadcast_to([128, 16, 16])
        cimb = cim.unsqueeze(2).broadcast_to([128, 16, 16])
        tt(bbr, braw, creb, ALU.mult, [bB, bw], [bB])
        tt(p1, biraw, cimb, ALU.mult, [bB, bw], [bB])
        tt(bbr, bbr, p1, ALU.subtract, [bB], [bB])
        tt(bbi, biraw, creb, ALU.mult, [bB, bw], [bB])
        tt(p1, braw, cimb, ALU.mult, [bB, bw], [bB])
        tt(bbi, bbi, p1, ALU.add, [bB], [bB])
        for (src, dstl) in ((bbr, Bre_l), (bbi, Bim_l)):
            memset(Wz, 0.0, [bWz])
            Wz4 = Wz.rearrange("p (g j) c -> p g j c", g=4)
            src4 = src.rearrange("p (g j) h -> p g j h", g=4)
            for jj in range(4):
                copy(Wz4[0:64, :, jj, 32 * jj:32 * jj + 16], src4[0:64, :, jj, :], [bB], [bWz])
                copy(Wz4[64:128, :, jj, 32 * jj + 16:32 * jj + 32], src4[64:128, :, jj, :], [bB], [bWz])
            for G in range(4):
                bi = G % 2
                for jj in range(4):
                    tr(PB[bi][:, jj * 128:(jj + 1) * 128], Wz[:, G * 4 + jj, :], ident[:], [bWz, bconst], [bPB[bi]],
                       sig=(jj == 3))
                copy(dstl[:, G * 4:(G + 1) * 4, :].rearrange("p a b -> p (a b)"), PB[bi], [bPB[bi]], [blhs], eng='act')
        for (srcd, dstl, sgn) in ((c_re, Cre_l, 1.0), (c_im, CimN_l, -1.0)):
            memset(Vz, 0.0, [bVz])
            for g2 in range(2):
                P.dma('sp', Vz[16 * g2:16 * g2 + 16, :, 64 * g2:64 * g2 + 64],
                      srcd[l].rearrange("(j g) h p -> g h j p", g=2)[g2], writes=[bVz])
            bi = 2
            for j in range(16):
                tr(PB[bi][:, j * 32:(j + 1) * 32], Vz[:, j, :], ident[0:32, 0:32], [bVz, bconst], [bPB[bi]], sig=(j == 15))
            act(dstl.rearrange("p a b -> p (a b)"), PB[bi], AF.Copy, [bPB[bi]], [blhs], scale=sgn)
            if dstl is Cre_l:
                act(CreN_l.rearrange("p a b -> p (a b)"), PB[bi], AF.Copy, [bPB[bi]], [blhs], scale=-1.0)
        A.off = scr0
        bscr = [bB, bWz, bVz, bang]
        P.barrier(['dve', 'act', 'pe', 'sp', 'pool'])
        hl_re = A.f32([16])
        hl_im = A.f32([16])
        bhl = [Buf() for _ in range(4)]
        memset(hl_re, 0.0, bhl)
        memset(hl_im, 0.0, bhl)
        NW = 4
        wk = []
        for i in range(NW):
            blk = A.f32([6, 4, 128])
            d_ = dict(ta=blk[:, 0], tb=blk[:, 1], zre=blk[:, 2], zim=blk[:, 3], qre=blk[:, 4], qim=blk[:, 5],
                      pb4=A.bf16([4, 4, 128]), blk=blk,
                      bta=Buf(), btb=Buf(), bzr=Buf(), bzi=Buf(), bq=Buf(), bhb=Buf())
            wk.append(d_)
        W0 = wk[0]
        yv = W0['blk'][:, 0].rearrange("p a b -> p (a b)")
        y2 = W0['blk'][:, 1].rearrange("p a b -> p (a b)")
        ysg = W0['blk'][:, 2].rearrange("p a b -> p (a b)")
        gsg = W0['blk'][:, 3].rearrange("p a b -> p (a b)")
        tw = W0['blk'][:, 4:6].rearrange("p a b c -> p (a b c)")
        bgsg = W0['bzi']
        btw = W0['bq']
        W1 = wk[1]
        yg = W1['blk'][:, 0:2].rearrange("p a b c -> p (a b c)").bitcast(BF16).rearrange("p (a b) -> p a b", a=4)
        byg = W1['bta']
        W1['btb'] = byg
        ys5 = W1['blk'][:, 2:4].rearrange("p a b c -> p (a b c)").bitcast(BF16).rearrange("p (a b) -> p a b", a=4)
        bys5 = W1['bzr']
        W1['bzi'] = bys5
        def unit(tt_, c, G, W, bk):
            cs = slice(tt_ * 512 + c * 128, tt_ * 512 + (c + 1) * 128)
            pre, pim = PB[bk], PB[bk + 1]
            bpre, bpim = bPB[bk], bPB[bk + 1]
            for j in range(4):
                mm(pre[:, j * 128:(j + 1) * 128], Bre_l[:, G * 4 + j, :], uT[:, G, cs], True, True,
                   [blhs, buT[tt_]], [bpre], sig=(j == 3))
            for j in range(4):
                mm(pim[:, j * 128:(j + 1) * 128], Bim_l[:, G * 4 + j, :], uT[:, G, cs], True, True,
                   [blhs, buT[tt_]], [bpim], sig=(j == 3))
            yield
            cG = ctab[:, G * 4:(G + 1) * 4, :]
            sG = stab[:, G * 4:(G + 1) * 4, :]
            pre3 = pre.rearrange("p (a b) -> p a b", a=4)
            pim3 = pim.rearrange("p (a b) -> p a b", a=4)
            bta, btb, bzr, bzi, bq = W['bta'], W['btb'], W['bzr'], W['bzi'], W['bq']
            tt(W['ta'], pre3, cG, ALU.mult, [bpre, btab], [bta])
            yield
            tt(W['tb'], pim3, sG, ALU.mult, [bpim, btab], [btb])
            yield
            tt(W['zre'], W['ta'], W['tb'], ALU.add, [bta, btb], [bzr])
            yield
            tt(W['ta'], pim3, cG, ALU.mult, [bpim, btab], [bta])
            yield
            tt(W['tb'], pre3, sG, ALU.mult, [bpre, btab], [btb])
            yield
            tt(W['zim'], W['ta'], W['tb'], ALU.subtract, [bta, btb], [bzi])
            yield
            for (zz, qq, hl, bz) in ((W['zre'], W['qre'], hl_re, bzr), (W['zim'], W['qim'], hl_im, bzi)):
                for j in range(4):
                    jj = G * 4 + j
                    P.op('dve', lambda e, zz=zz, qq=qq, hl=hl, j=j, jj=jj: e.tensor_tensor_scan(
                        out=qq[:, j, :], data0=rr[:, jj:jj + 1].broadcast_to([128, 128]), data1=zz[:, j, :],
                        initial=hl[:, jj:jj + 1], op0=ALU.mult, op1=ALU.add),
                        reads=[bz, bw, bhl[G]], writes=[bq], sig=(j == 3))
                yield
            pb4 = W['pb4']
            prods = ((W['ta'], W['qre'], cG, bta), (W['tb'], W['qim'], sG, btb),
                     (W['zre'], W['qre'], sG, bzr), (W['zim'], W['qim'], cG, bzi))
            for pi, (dst, qq, tab, bd) in enumerate(prods):
                tt(dst, qq, tab, ALU.mult, [bq, btab], [bd])
                copy(pb4[:, pi], dst, [bd], [W['bhb']], eng='act')
                yield
            tt(hl_re[:, G * 4:(G + 1) * 4], W['ta'][:, :, 127], W['tb'][:, :, 127], ALU.subtract, [bta, btb], [bhl[G]])
            yield
            tt(hl_im[:, G * 4:(G + 1) * 4], W['zre'][:, :, 127], W['zim'][:, :, 127], ALU.add, [bzr, bzi], [bhl[G]])
            yield
            lhs = (Cre_l, CreN_l, CimN_l, CimN_l)
            for j in range(4):
                for pi in range(4):
                    mm(PB[G][32 * j:32 * j + 32, c * 128:(c + 1) * 128], lhs[pi][:, G * 4 + j, :], pb4[:, pi, j, :],
                       pi == 0, pi == 3, [blhs, W['bhb']], [bPB[G]], sig=(j == 3 and pi == 3), tile_position=(0, 32 * j))

        wi = 0
        for tt_ in range(4):
            hs = slice(tt_ * 512, (tt_ + 1) * 512)
            for c in range(4):
                for Gp in (0, 2):
                    gens = [unit(tt_, c, Gp, wk[wi % NW], 4), unit(tt_, c, Gp + 1, wk[(wi + 1) % NW], 6)]
                    wi += 2
                    while gens:
                        for g_ in list(gens):
                            try:
                                next(g_)
                            except StopIteration:
                                gens.remove(g_)
            for G in range(4):
                stt(yv, uT[:, G, hs], small[:, 8 + G:9 + G], PB[G], ALU.mult, ALU.add, [buT[tt_], bsmall, bPB[G]],
                    [W0['bta']])
                act(y2, yv, AF.Square, [W0['bta']], [W0['btb']])
                ts(y2, y2, 0.044715, ALU.mult, [W0['btb']], [W0['btb']], s2=1.0, op1=ALU.add)
                tt(y2, y2, yv, ALU.mult, [W0['btb'], W0['bta']], [W0['btb']])
                act(ysg, y2, AF.Sigmoid, [W0['btb']], [W0['bzr']], scale=1.5957691216057308)
                tt(yg[:, G, :], yv, ysg, ALU.mult, [W0['bta'], W0['bzr']], [byg])
            for m in range(4):
                bi = 4 + m % 2
                for k in range(4):
                    mm(PB[bi], wgl[:, k, m * 128:(m + 1) * 128], yg[:, k, :], k == 0, k == 3, [bwgl, byg], [bPB[bi]])
                act(gsg, PB[bi], AF.Sigmoid, [bPB[bi], bsmall], [bgsg], bias=small[:, 12 + m:13 + m])
                tt(ys5[:, m, :], yg[:, m, :], gsg, ALU.mult, [byg, bgsg], [bys5])
            if DBG and tt_ == 0 and l == 0 and s == 0:
                dbg_dump("ys5", ys5, [128, 4, 512], bys5, BF16)
            for sub in range(4):
                t = tt_ * 4 + sub
                pp = 2 + sub % 2
                for half in range(2):
                    bi = pp * 2 + half
                    for k in range(4):
                        mm(PB[bi], ys5[:, k, sub * 128:(sub + 1) * 128], wo[:, k, half * 512:(half + 1) * 512],
                           k == 0, k == 3, [bys5, bwo], [bPB[bi]])
                tt(tw, pst[pp][:, :], gt_bc[:], ALU.mult, [bPB[pp * 2], bPB[pp * 2 + 1], bgt], [btw])
                tt(x_sb[:, t, :], x_sb[:, t, :], tw, ALU.add, [bx[t], btw], [bx[t]])
                layer_norm_tile(t, gb_eng=LN_ENG)

    def ffn(l, s):
        A.reset()
        moe = (l % 2 == 1)
        nexp = NE if moe else 1
        i_l = l // 2
        wg_d = moe_wg[i_l] if moe else ffn_wg[i_l]
        wu_d = moe_wu[i_l] if moe else ffn_wu[i_l]
        wd_d = moe_wd[i_l] if moe else ffn_wd[i_l]
        P.dma('sp', gt_bc[:], gt_scr[l, 1, s].partition_broadcast(128), writes=[bgt])
        load_bcast(g_bc, bgb, ln_ffn_g[l])
        load_bcast(b_bc, bbb, ln_ffn_b[l])
        hT = A.bf16([8, SEQ])
        bhT = [Buf() for _ in range(4)]
        NWB = 3
        wgt = [A.bf16([8, 256]) for _ in range(NWB)]
        wut = [A.bf16([8, 256]) for _ in range(NWB)]
        wdt = [A.bf16([2, D]) for _ in range(NWB)]
        bwgu = [Buf() for _ in range(NWB)]
        bwd = [Buf() for _ in range(NWB)]
        actt = [A.bf16([2, 512]) for _ in range(2)]
        bact = [Buf(), Buf()]
        sgt = [A.f32([512]) for _ in range(2)]
        bsgt = [Buf(), Buf()]
        comb = A.f32([16, NE])
        bcomb = Buf()
        if moe:
            h2f = A.f32([8, 512])
            bh2f = Buf()
            wr = A.f32([8, NE])
            bwr = Buf()
            P.dma('sp', wr, w_router[i_l].rearrange("(k p) e -> p k e", p=128), writes=[bwr])
            brt = A.f32([NE])
            P.dma('sp', brt, b_router[i_l].partition_broadcast(128), writes=[bwr])
            rw = A.f32([8, NE])
            brw = Buf()
        def prologue(tt_):
            make_hT(l, 4, 3, tt_, hT[:, :, tt_ * 512:(tt_ + 1) * 512], bhT[tt_],
                    f32_copy=(h2f if moe else None), bf32=(bh2f if moe else None))
            for sub in range(4):
                t = tt_ * 4 + sub
                if moe:
                    bi = 2 + sub % 2
                    for k in range(8):
                        mm(PB[bi][:, 0:NE], h2f[:, k, sub * 128:(sub + 1) * 128], wr[:, k, :], k == 0, k == 7,
                           [bh2f, bwr], [bPB[bi]])
                    lg = rw[:, 0, :]
                    tt(lg, PB[bi][:, 0:NE], brt, ALU.add, [bPB[bi], bwr], [brw])
                    m1 = rw[:, 1, 0:1]
                    P.op('dve', lambda e, lg=lg, m1=m1: e.reduce_max(out=m1, in_=lg, axis=mybir.AxisListType.X),
                         reads=[brw], writes=[brw])
                    eq = rw[:, 2, :]
                    ts(eq, lg, m1, ALU.is_equal, [brw], [brw])
                    l2 = rw[:, 3, :]
                    stt(l2, eq, -1e30, lg, ALU.mult, ALU.add, [brw], [brw])
                    m2 = rw[:, 1, 1:2]
                    P.op('dve', lambda e, l2=l2, m2=m2: e.reduce_max(out=m2, in_=l2, axis=mybir.AxisListType.X),
                         reads=[brw], writes=[brw])
                    sel = rw[:, 4, :]
                    ts(sel, lg, m2, ALU.is_ge, [brw], [brw])
                    nm1 = rw[:, 1, 2:3]
                    ts(nm1, m1, -1.0, ALU.mult, [brw], [brw])
                    ex = rw[:, 5, :]
                    act(ex, lg, AF.Exp, [brw], [brw], bias=nm1)
                    tt(ex, ex, sel, ALU.mult, [brw], [brw])
                    ssum = rw[:, 1, 3:4]
                    P.op('dve', lambda e, ex=ex, ssum=ssum: e.reduce_sum(out=ssum, in_=ex, axis=mybir.AxisListType.X),
                         reads=[brw], writes=[brw])
                    P.op('dve', lambda e, ssum=ssum: e.reciprocal(out=ssum, in_=ssum), reads=[brw], writes=[brw])
                    ts(comb[:, t, :], ex, ssum, ALU.mult, [brw], [bcomb])
                ts(x_sb[:, t, :], x_sb[:, t, :], ALPHA, ALU.mult, [bx[t]], [bx[t]])
        NFG = DFF // 256
        pieces = [(e, fg) for e in range(nexp) for fg in range(NFG)]

        def load_piece(i):
            e, fg = pieces[i]
            bi = i % NWB
            P.dma('pool', wgt[bi], wg_d[e, :, fg * 256:(fg + 1) * 256].rearrange("(k p) n -> p k n", p=128),
                  writes=[bwgu[bi]])
            P.dma('pool', wut[bi], wu_d[e, :, fg * 256:(fg + 1) * 256].rearrange("(k p) n -> p k n", p=128),
                  writes=[bwgu[bi]])
            P.dma('pool', wdt[bi], wd_d[e, fg * 256:(fg + 1) * 256, :].rearrange("(k p) n -> p k n", p=128),
                  writes=[bwd[bi]])
            P.op('pool', lambda en: en.tensor_tensor(out=wdt[bi], in0=wdt[bi],
                                                     in1=gt_bc[:].unsqueeze(1).broadcast_to([128, 2, D]), op=ALU.mult),
                 reads=[bwd[bi], bgt], writes=[bwd[bi]])

        pend = []

        def down(i, tt_, ai):
            e, fg = pieces[i]
            bi = i % NWB
            for sub in range(4):
                t = tt_ * 4 + sub
                pp = 2 + sub % 2
                for half in range(2):
                    pbi = pp * 2 + half
                    for fc in range(2):
                        mm(PB[pbi], actt[ai][:, fc, sub * 128:(sub + 1) * 128], wdt[bi][:, fc, half * 512:(half + 1) * 512],
                           fc == 0, fc == 1, [bact[ai], bwd[bi]], [bPB[pbi]])
                sc = comb[:, t, e:e + 1] if moe else 1.0
                stt(x_sb[:, t, :], pst[pp][:, :], sc, x_sb[:, t, :], ALU.mult, ALU.add,
                    [bPB[pp * 2], bPB[pp * 2 + 1], bcomb, bx[t]], [bx[t]])
                if i == len(pieces) - 1:
                    layer_norm_tile(t)
                    if l == N_LAYER - 1:
                        P.dma('sp', out_d[s, t * 128:(t + 1) * 128, :], x_sb[:, t, :], reads=[bx[t]], writes=[Buf()])

        load_piece(0)
        if len(pieces) > 1:
            load_piece(1)
        prologue(0)
        ai = 0
        for i in range(len(pieces)):
            bi = i % NWB
            for tt_ in range(4):
                hs = slice(tt_ * 512, (tt_ + 1) * 512)
                a = ai % 2
                ai += 1
                for fc in range(2):
                    gb, ubk = fc * 2, fc * 2 + 1
                    for k in range(8):
                        mm(PB[gb], wgt[bi][:, k, fc * 128:(fc + 1) * 128], hT[:, k, hs], k == 0, k == 7,
                           [bwgu[bi], bhT[tt_]], [bPB[gb]])
                    for k in range(8):
                        mm(PB[ubk], wut[bi][:, k, fc * 128:(fc + 1) * 128], hT[:, k, hs], k == 0, k == 7,
                           [bwgu[bi], bhT[tt_]], [bPB[ubk]])
                    act(sgt[fc], PB[gb], AF.Silu, [bPB[gb]], [bsgt[fc]])
                    tt(actt[a][:, fc, :], PB[ubk], sgt[fc], ALU.mult, [bPB[ubk], bsgt[fc]], [bact[a]])
                if pend:
                    down(*pend.pop(0))
                if tt_ == 0 and i + 2 < len(pieces):
                    load_piece(i + 2)
                if i == 0 and tt_ + 1 < 4:
                    prologue(tt_ + 1)
                pend.append((i, tt_, a))
        while pend:
            down(*pend.pop(0))
        if DBG and l == 1 and s == 0 and moe:
            dbg_dump("comb", comb, [128, 16, NE], bcomb)

    for s in range(N_SEQ):
        cur_s[0] = s
        if s > 0:
            load_x(s)
        for l in range(N_LAYER):
            uT, buT = pass_a(l, s)
            if DBG and STOP_AFTER == "A":
                break
            P.barrier()
            pass_b(l, s, uT, buT)
            if DBG and STOP_AFTER == "B":
                break
            P.barrier()
            ffn(l, s)
            P.barrier()
        if DBG and STOP_AFTER:
            for t in range(16):
                P.dma('sp', out_d[s, t * 128:(t + 1) * 128, :], x_sb[:, t, :], reads=[bx[t]], writes=[Buf()])
    P.finish()
    return nc, P, list(dbg_outs)


_CACHE = {}


def _consts():
    ident = np.eye(128, dtype=np.float32)
    j = np.arange(128)
    mask = (j[:, None] <= j[None, :]).astype(np.float32)
    m0 = np.ones((128, 512), np.float32)
    m0[:, ::128] = 0.0
    tau = np.tile(np.arange(1, 129, dtype=np.float32)[None, :], (128, 1))
    return dict(k_ident=ident, k_mask=mask, k_m0=m0, k_tau=tau)


def kernel(**inputs):
    if "nc" not in _CACHE:
        _CACHE["nc"] = build_program()
    nc, P, dbg_names = _CACHE["nc"]
    f = lambda k: np.ascontiguousarray(np.asarray(inputs[k], dtype=np.float32))
    x = f("x")
    c = f("c")
    shared = {}
    for k in ["mod_w", "mod_b", "w_in", "w_out", "s5_log_dt", "s5_c_re", "s5_c_im", "s5_d", "s5_w_glu", "s5_b_glu",
              "gla_w_alpha_up", "gla_b_alpha", "gla_head_gain", "ln_mix_g", "ln_mix_b", "moe_w_router", "moe_b_router",
              "moe_w_gate", "moe_w_up", "moe_w_down", "ln_ffn_g", "ln_ffn_b"]:
        shared[k] = f(k)
    for k in ["s5_lam_re", "s5_lam_im"]:
        shared[k] = f(k).reshape(DEPTH, 2048)
    for k in ["s5_b_re", "s5_b_im"]:
        shared[k] = f(k).reshape(DEPTH, 32 * 64 * 16)
    for k in ["ffn_w_gate", "ffn_w_up", "ffn_w_down"]:
        a = f(k)
        shared[k] = a.reshape((1, 1) + a.shape[1:])
    shared.update(_consts())
    in_maps = []
    for i in range(NCORES):
        m = dict(shared)
        m["x"] = np.ascontiguousarray(x[i * SPC:(i + 1) * SPC])
        ci = c[i * SPC:(i + 1) * SPC]
        m["cT"] = np.ascontiguousarray(ci.T.reshape(8, 128, SPC).transpose(1, 0, 2))
        in_maps.append(m)
    res = run_bass_kernel_spmd(nc, in_maps, core_ids=list(range(NCORES)))
    out = np.concatenate([np.asarray(r["out"]) for r in res.results], axis=0).astype(np.float32)
    if DBG:
        _CACHE["dbg"] = [{n: np.asarray(r["dbg_" + n]) for n in dbg_names} for r in res.results]
    return out
```
